# Optimizing a Trainium2 kernel written in Bass

```python
import math
import jax
import jax.numpy as jnp
from jax import lax
import numpy as np

D_MODEL = 1024
BATCH = 16
SEQ = 4096
DEPTH = 4

GRID_W = 64
N_MIXERS = 3
HEAD_DIM = 64
MIX_WIDTH = 768
MIX_HEADS = MIX_WIDTH // HEAD_DIM
MEM_LEN = 256
MEM_HEADS = 4
MEM_WIDTH = MEM_HEADS * HEAD_DIM
CAT_WIDTH = MIX_WIDTH + MEM_WIDTH

NA_KH_MAX = 8
NA_KW = 16

KV_HEADS = 4
KV_WIDTH = KV_HEADS * HEAD_DIM
ROPE_THETA = 10000.0
Q_BLOCK = 128
QK_NORM_EPS = 1e-6

HYENA_ORDER = 2
HYENA_BANDS = 16
HYENA_EMB = 2 * HYENA_BANDS + 1
HYENA_FFN = 64
HYENA_WIDTH = (HYENA_ORDER + 1) * MIX_WIDTH
HYENA_DECAY_TARGET = 1e-2
HYENA_FAST_DECAY = 0.3
HYENA_SLOW_DECAY = 1.5

N_EXPERTS = 32
TOP_K = 4
D_EXPERT = D_MODEL
SWIGLU_LIMIT = 7.0
SWIGLU_ALPHA = 1.702

DEEPNORM_ALPHA = (2 * DEPTH) ** 0.25
DEEPNORM_BETA = (8 * DEPTH) ** -0.25
LN_EPS = 1e-5

N_LAYERS_A = len(range(0, DEPTH, N_MIXERS))
N_LAYERS_B = len(range(1, DEPTH, N_MIXERS))
N_LAYERS_C = len(range(2, DEPTH, N_MIXERS))
IN_WIDTH_A = 3 * MIX_WIDTH + MEM_WIDTH
IN_WIDTH_B = MIX_WIDTH + 2 * KV_WIDTH + MEM_WIDTH
IN_WIDTH_C = HYENA_WIDTH + MEM_WIDTH

kernel_name = 'hybrid_na_gqa_hyena_moe_encoder'

F32 = jnp.float32


def layer_norm(x, g, b):
    xf = x.astype(F32)
    mu = jnp.mean(xf, -1, keepdims=True)
    xc = xf - mu
    var = jnp.mean(xc * xc, -1, keepdims=True)
    return (xc * lax.rsqrt(var + LN_EPS) * g.astype(F32) + b.astype(F32)).astype(x.dtype)


def rms_norm(x, g):
    xf = x.astype(F32)
    inv = lax.rsqrt(jnp.mean(xf * xf, -1, keepdims=True) + QK_NORM_EPS)
    return (xf * inv * g.astype(F32)).astype(x.dtype)


def split_heads(t, n_heads):
    return t.reshape(*t.shape[:-1], n_heads, HEAD_DIM)


def neighbourhood_attention(q, k, v, rpb):
    B, L, H, Dh = q.shape
    rows = L // GRID_W
    kh = min(NA_KH_MAX, rows)
    qg = q.reshape(B, rows, GRID_W, H, Dh)
    kg = k.reshape(B, rows, GRID_W, H, Dh)
    vg = v.reshape(B, rows, GRID_W, H, Dh)
    col = np.arange(GRID_W)
    col_start = np.clip(col - NA_KW // 2, 0, GRID_W - NA_KW)
    col_idx = col_start[:, None] + np.arange(NA_KW)[None, :]
    col_off = col_idx - col[:, None] + NA_KW - 1
    rpb_cols = rpb.astype(F32)[:, :, col_off]
    scale = Dh ** -0.5

    def row_block(r):
        r0 = jnp.clip(r - kh // 2, 0, rows - kh)
        k_band = lax.dynamic_slice_in_dim(kg, r0, kh, axis=1)
        v_band = lax.dynamic_slice_in_dim(vg, r0, kh, axis=1)
        k_win = k_band[:, :, col_idx]
        v_win = v_band[:, :, col_idx]
        q_row = lax.dynamic_index_in_dim(qg, r, axis=1, keepdims=False)
        row_off = r0 + jnp.arange(kh) - r + NA_KH_MAX - 1
        bias = jnp.take(rpb_cols, row_off, axis=1).transpose(0, 2, 1, 3)
        s = jnp.einsum('bqhd,biqjhd->bhqij', q_row, k_win).astype(F32) * scale + bias[None]
        p = jax.nn.softmax(s.reshape(B, H, GRID_W, kh * NA_KW), axis=-1)
        p = p.reshape(B, H, GRID_W, kh, NA_KW).astype(v.dtype)
        return jnp.einsum('bhqij,biqjhd->bqhd', p, v_win)

    out = lax.map(row_block, jnp.arange(rows))
    return out.transpose(1, 0, 2, 3, 4).reshape(B, L, H, Dh)


def axial_rope_tables(L):
    pos = jnp.arange(L)
    row = (pos // GRID_W).astype(F32)
    col = (pos % GRID_W).astype(F32)
    axis_dim = HEAD_DIM // 2
    inv_freq = ROPE_THETA ** (-jnp.arange(0, axis_dim, 2, dtype=F32) / axis_dim)
    ang = jnp.concatenate([row[:, None] * inv_freq, col[:, None] * inv_freq], -1)
    return jnp.cos(ang), jnp.sin(ang)


def apply_rope(x, cos, sin):
    xf = x.astype(F32).reshape(*x.shape[:-1], HEAD_DIM // 2, 2)
    x1, x2 = xf[..., 0], xf[..., 1]
    c = cos[None, :, None, :]
    s = sin[None, :, None, :]
    out = jnp.stack([x1 * c - x2 * s, x1 * s + x2 * c], -1).reshape(x.shape)
    return out.astype(x.dtype)


def gqa_blocked(q, k, v):
    B, L, Hq, Dh = q.shape
    G = Hq // KV_HEADS
    nblk = L // Q_BLOCK
    qb = q.reshape(B, nblk, Q_BLOCK, KV_HEADS, G, Dh).transpose(1, 0, 2, 3, 4, 5)
    scale = Dh ** -0.5

    def block(q_blk):
        s = jnp.einsum('bqkgd,bskd->bkgqs', q_blk, k).astype(F32) * scale
        p = jax.nn.softmax(s, axis=-1).astype(v.dtype)
        return jnp.einsum('bkgqs,bskd->bqkgd', p, v)

    out = lax.map(block, qb)
    return out.transpose(1, 0, 2, 3, 4, 5).reshape(B, L, Hq, Dh)


def short_conv_centred(u, w, b):
    up = jnp.pad(u, ((0, 0), (1, 1), (0, 0)))
    return up[:, :-2] * w[0] + up[:, 1:-1] * w[1] + up[:, 2:] * w[2] + b


def hyena_filters(L, w1, b1, w2, b2, w3, b3, w4, b4, freq):
    pos = jnp.arange(L, dtype=F32)
    t = pos / max(L - 1, 1)
    w = 2.0 * math.pi * pos / L
    f = jnp.linspace(1e-4, HYENA_BANDS - 1, HYENA_BANDS, dtype=F32)
    wf = w[:, None] * f[None, :]
    z = jnp.concatenate([t[:, None], jnp.cos(wf), -jnp.sin(wf)], -1)
    fr = freq.astype(F32)
    h = jnp.sin(fr * (z @ w1.astype(F32) + b1.astype(F32)))
    h = jnp.sin(fr * (h @ w2.astype(F32) + b2.astype(F32)))
    h = jnp.sin(fr * (h @ w3.astype(F32) + b3.astype(F32)))
    h = (h @ w4.astype(F32) + b4.astype(F32)).reshape(L, 2, HYENA_ORDER, MIX_WIDTH)
    max_decay = math.log(HYENA_DECAY_TARGET) / HYENA_FAST_DECAY
    min_decay = math.log(HYENA_DECAY_TARGET) / HYENA_SLOW_DECAY
    deltas = jnp.abs(jnp.linspace(min_decay, max_decay, MIX_WIDTH, dtype=F32))
    h = h * jnp.exp(-t[:, None, None, None] * deltas)
    return h / jnp.sum(jnp.abs(h), axis=(0, 1), keepdims=True)


def bidir_long_conv(u, h_fwd, h_bwd, bias):
    L, C = h_fwd.shape
    g = jnp.concatenate([h_fwd, jnp.zeros((1, C), F32), h_bwd[:0:-1]], 0)
    n = 2 * L
    u_f = jnp.fft.rfft(u.astype(F32), n=n, axis=1)
    g_f = jnp.fft.rfft(g, n=n, axis=0)
    y = jnp.fft.irfft(u_f * g_f[None], n=n, axis=1)[:, :L]
    return (y + u.astype(F32) * bias.astype(F32)).astype(u.dtype)


def hyena_mixer(u, conv_w, conv_b, filt, long_bias):
    uc = short_conv_centred(u, conv_w, conv_b)
    parts = jnp.split(uc, HYENA_ORDER + 1, axis=-1)
    z = parts[0]
    for o in range(HYENA_ORDER):
        z = parts[o + 1] * bidir_long_conv(z, filt[:, 0, o], filt[:, 1, o], long_bias[o])
    return z


def memory_attention(q, k, v):
    s = jnp.einsum('blhd,bmhd->bhlm', q, k).astype(F32) * (HEAD_DIM ** -0.5)
    p = jax.nn.softmax(s, axis=-1).astype(v.dtype)
    return jnp.einsum('bhlm,bmhd->blhd', p, v)


def moe(x, router_w, router_b, w1, b1, w2, b2):
    B, L, D = x.shape
    xt = x.reshape(-1, D)
    logits = (xt @ router_w + router_b).astype(F32)
    top_val, top_idx = lax.top_k(logits, TOP_K)
    gate = jax.nn.softmax(top_val, axis=-1)
    comb = jnp.einsum('nk,nke->ne', gate, jax.nn.one_hot(top_idx, N_EXPERTS, dtype=F32)).astype(x.dtype)
    y = jnp.zeros_like(xt)
    for e in range(N_EXPERTS):
        h = xt @ w1[e] + b1[e]
        g = jnp.minimum(h[:, :D_EXPERT], SWIGLU_LIMIT)
        u = jnp.clip(h[:, D_EXPERT:], -SWIGLU_LIMIT, SWIGLU_LIMIT)
        a = (u + 1.0) * (g * jax.nn.sigmoid(SWIGLU_ALPHA * g))
        y = y + comb[:, e:e + 1] * (a @ w2[e] + b2[e])
    return y.reshape(B, L, D)


def setup_inputs(seed: int = 0) -> dict:
    key = jax.random.key(seed)
    ks = iter(jax.random.split(key, 40))

    def nrm(shape, scale):
        return jax.random.normal(next(ks), shape, F32) * scale

    D = D_MODEL
    return {
        'x': nrm((BATCH, SEQ, D), 1.0),
        'mem': nrm((BATCH, MEM_LEN, D), 1.0),
        'w_in_a': nrm((N_LAYERS_A, D, IN_WIDTH_A), D ** -0.5),
        'rpb_a': nrm((N_LAYERS_A, MIX_HEADS, 2 * NA_KH_MAX - 1, 2 * NA_KW - 1), 0.1),
        'w_in_b': nrm((N_LAYERS_B, D, IN_WIDTH_B), D ** -0.5),
        'q_norm_b': 1.0 + nrm((N_LAYERS_B, HEAD_DIM), 0.02),
        'k_norm_b': 1.0 + nrm((N_LAYERS_B, HEAD_DIM), 0.02),
        'w_in_c': nrm((N_LAYERS_C, D, IN_WIDTH_C), D ** -0.5),
        'conv_w_c': nrm((N_LAYERS_C, 3, HYENA_WIDTH), 3 ** -0.5),
        'conv_b_c': nrm((N_LAYERS_C, HYENA_WIDTH), 0.02),
        'filt_w1': nrm((N_LAYERS_C, HYENA_EMB, HYENA_FFN), HYENA_EMB ** -0.5),
        'filt_b1': nrm((N_LAYERS_C, HYENA_FFN), 0.02),
        'filt_w2': nrm((N_LAYERS_C, HYENA_FFN, HYENA_FFN), HYENA_FFN ** -0.5),
        'filt_b2': nrm((N_LAYERS_C, HYENA_FFN), 0.02),
        'filt_w3': nrm((N_LAYERS_C, HYENA_FFN, HYENA_FFN), HYENA_FFN ** -0.5),
        'filt_b3': nrm((N_LAYERS_C, HYENA_FFN), 0.02),
        'filt_w4': nrm((N_LAYERS_C, HYENA_FFN, 2 * HYENA_ORDER * MIX_WIDTH), HYENA_FFN ** -0.5),
        'filt_b4': nrm((N_LAYERS_C, 2 * HYENA_ORDER * MIX_WIDTH), 0.02),
        'filt_freq': 1.0 + nrm((N_LAYERS_C, HYENA_FFN), 0.02),
        'long_bias_c': nrm((N_LAYERS_C, HYENA_ORDER, MIX_WIDTH), 1.0),
        'w_mem_kv': nrm((DEPTH, D, 2 * MEM_WIDTH), D ** -0.5),
        'w_out': nrm((DEPTH, CAT_WIDTH, D), CAT_WIDTH ** -0.5 * DEEPNORM_BETA),
        'ln1_g': 1.0 + nrm((DEPTH, D), 0.02),
        'ln1_b': nrm((DEPTH, D), 0.02),
        'ln2_g': 1.0 + nrm((DEPTH, D), 0.02),
        'ln2_b': nrm((DEPTH, D), 0.02),
        'router_w': nrm((DEPTH, D, N_EXPERTS), D ** -0.5),
        'router_b': nrm((DEPTH, N_EXPERTS), 0.01),
        'moe_w1': nrm((DEPTH, N_EXPERTS, D, 2 * D_EXPERT), D ** -0.5),
        'moe_b1': nrm((DEPTH, N_EXPERTS, 2 * D_EXPERT), 0.02),
        'moe_w2': nrm((DEPTH, N_EXPERTS, D_EXPERT, D), D_EXPERT ** -0.5 * DEEPNORM_BETA),
        'moe_b2': nrm((DEPTH, N_EXPERTS, D), 0.02),
    }


def reference(x, mem, w_in_a, rpb_a, w_in_b, q_norm_b, k_norm_b, w_in_c, conv_w_c, conv_b_c,
              filt_w1, filt_b1, filt_w2, filt_b2, filt_w3, filt_b3, filt_w4, filt_b4, filt_freq,
              long_bias_c, w_mem_kv, w_out, ln1_g, ln1_b, ln2_g, ln2_b, router_w, router_b,
              moe_w1, moe_b1, moe_w2, moe_b2):
    B, L, _ = x.shape
    for i in range(DEPTH):
        kind = i % N_MIXERS
        j = i // N_MIXERS
        if kind == 0:
            proj = x @ w_in_a[j]
            q, k, v, q_mem = jnp.split(proj, [MIX_WIDTH, 2 * MIX_WIDTH, 3 * MIX_WIDTH], axis=-1)
            mix = neighbourhood_attention(split_heads(q, MIX_HEADS), split_heads(k, MIX_HEADS),
                                          split_heads(v, MIX_HEADS), rpb_a[j])
        elif kind == 1:
            proj = x @ w_in_b[j]
            q, k, v, q_mem = jnp.split(proj, [MIX_WIDTH, MIX_WIDTH + KV_WIDTH, MIX_WIDTH + 2 * KV_WIDTH], axis=-1)
            cos, sin = axial_rope_tables(L)
            q = apply_rope(rms_norm(split_heads(q, MIX_HEADS), q_norm_b[j]), cos, sin)
            k = apply_rope(rms_norm(split_heads(k, KV_HEADS), k_norm_b[j]), cos, sin)
            mix = gqa_blocked(q, k, split_heads(v, KV_HEADS))
        else:
            proj = x @ w_in_c[j]
            u, q_mem = jnp.split(proj, [HYENA_WIDTH], axis=-1)
            filt = hyena_filters(L, filt_w1[j], filt_b1[j], filt_w2[j], filt_b2[j], filt_w3[j],
                                 filt_b3[j], filt_w4[j], filt_b4[j], filt_freq[j])
            mix = hyena_mixer(u, conv_w_c[j], conv_b_c[j], filt, long_bias_c[j])
        k_mem, v_mem = jnp.split(mem @ w_mem_kv[i], 2, axis=-1)
        mem_out = memory_attention(split_heads(q_mem, MEM_HEADS), split_heads(k_mem, MEM_HEADS),
                                   split_heads(v_mem, MEM_HEADS))
        cat = jnp.concatenate([mix.reshape(B, L, MIX_WIDTH), mem_out.reshape(B, L, MEM_WIDTH)], axis=-1)
        x = layer_norm(DEEPNORM_ALPHA * x + cat @ w_out[i], ln1_g[i], ln1_b[i])
        y = moe(x, router_w[i], router_b[i], moe_w1[i], moe_b1[i], moe_w2[i], moe_b2[i])
        x = layer_norm(DEEPNORM_ALPHA * x + y, ln2_g[i], ln2_b[i])
    return x
```

```python
import math
from contextlib import ExitStack
import numpy as np
import ml_dtypes
import concourse.bass as bass
import concourse.mybir as mybir
from concourse.bass_utils import run_bass_kernel_spmd

F32 = mybir.dt.float32
BF16 = mybir.dt.bfloat16
U8 = mybir.dt.uint8
AF = mybir.ActivationFunctionType
ALU = mybir.AluOpType
AX = mybir.AxisListType

NCORES = 8
D = 1024
SEQ = 4096
NB = 2
NT = NB * SEQ
DEPTH = 4
NE = 32
DE = 1024
ALPHA = (2 * DEPTH) ** 0.25
LN_EPS = 1e-5
TT = 512
ENGS = ("sync", "act", "dve", "pool", "pe")

CFG = {"layers": [0, 1, 2, 3], "n_experts": NE, "dump": None, "moe": True, "ses": True}


class Op:
    __slots__ = ("eng", "fn", "deps", "dma", "sig", "sem", "val", "idx", "prewait")

    def __init__(self, eng, fn, dma):
        self.eng = eng
        self.fn = fn
        self.dma = dma
        self.deps = []
        self.sig = False
        self.sem = None
        self.val = 0
        self.prewait = None


class Prog:
    def __init__(self, nc, same_engine_sync=True):
        self.nc = nc
        self.ops = []
        self.last_w = {}
        self.readers = {}
        self.ses = same_engine_sync
        self.last_op = {}
        self.dma_since = []

    def op(self, eng, fn, reads=(), writes=(), dma=False, extra_deps=()):
        o = Op(eng, fn, dma)
        o.idx = len(self.ops)
        pbr = [k for k in reads if k.startswith("pb")]
        if pbr:
            writes = list(writes) + pbr
        deps = {}
        for k in reads:
            w = self.last_w.get(k)
            if w is not None:
                deps[w.idx] = w
        for k in writes:
            w = self.last_w.get(k)
            if w is not None:
                deps[w.idx] = w
            for r in self.readers.get(k, ()):
                deps[r.idx] = r
        for d in extra_deps:
            deps[d.idx] = d
        for d in deps.values():
            if d.eng == eng and not d.dma:
                if eng == "pe" or not self.ses:
                    continue
            o.deps.append(d)
            d.sig = True
        for k in reads:
            self.readers.setdefault(k, []).append(o)
        for k in writes:
            self.last_w[k] = o
            self.readers[k] = []
        self.ops.append(o)
        self.last_op[eng] = o
        if dma:
            self.dma_since.append(o)
        return o

    def dma(self, out, in_, reads=(), writes=(), eng="sync", **kw):
        return self.op(eng, lambda e: e.dma_start(out=out, in_=in_, **kw), reads, writes, dma=True)

    def barrier(self):
        lasts = [o for o in self.last_op.values() if not o.dma]
        dmas = list(self.dma_since)
        fl = []
        for e in ENGS:
            fl.append(self.op(e, None, extra_deps=lasts + dmas))
        self.last_w = {}
        self.readers = {}
        self.dma_since = []
        for f in fl:
            pass

    def emit(self, sems):
        nc = self.nc
        cnt = {e: 0 for e in ENGS}
        dcnt = {q: [0] * len(v) for q, v in sems["dma"].items()}
        drr = {q: 0 for q in sems["dma"]}
        for o in self.ops:
            if o.dma:
                pool = sems["dma"][o.eng]
                si = drr[o.eng] % len(pool)
                drr[o.eng] += 1
                o.sem = pool[si]
                c = dcnt[o.eng][si]
                o.prewait = (o.sem, c * 16) if c > 0 else None
                dcnt[o.eng][si] = c + 1
                o.val = (c + 1) * 16
                o.sig = True
            elif o.sig and o.fn is not None:
                cnt[o.eng] += 1
                o.sem = sems[o.eng]
                o.val = cnt[o.eng]
            elif o.fn is None:
                o.sem = sems.get(o.eng)
                o.val = cnt[o.eng] if o.sem is not None else 0
        self.counts = dict(cnt)
        by_eng = {e: [o for o in self.ops if o.eng == e] for e in ENGS}
        with nc.Block() as block:
            def run(ename):
                def body(eng):
                    waited = {}
                    for o in by_eng[ename]:
                        waits = {}
                        if o.prewait is not None:
                            waits[id(o.prewait[0])] = o.prewait
                        for d in o.deps:
                            if d.val == 0:
                                continue
                            cur = waits.get(id(d.sem))
                            if cur is None or cur[1] < d.val:
                                waits[id(d.sem)] = (d.sem, d.val)
                        for key, (s, v) in waits.items():
                            if waited.get(key, 0) >= v:
                                continue
                            eng.wait_ge(s, v)
                            waited[key] = v
                        if o.fn is None:
                            continue
                        ins = o.fn(eng)
                        if o.sig:
                            ins.then_inc(o.sem, 16 if o.dma else 1)
                return body
            block.sync(run("sync"))
            block.scalar(run("act"))
            block.vector(run("dve"))
            block.gpsimd(run("pool"))
            block.tensor(run("pe"))


class Arena:
    def __init__(self, ap_u8, size):
        self.ap = ap_u8
        self.size = size
        self.base = 0
        self.off = 0

    def mark(self):
        self.base = self.off

    def reset(self):
        self.off = self.base

    def alloc(self, shape, dt):
        esz = 4 if dt == F32 else 2
        n = int(np.prod(shape)) * esz
        self.off = (self.off + 63) // 64 * 64
        a = self.off
        self.off += n
        assert self.off <= self.size, ("SBUF arena overflow", self.off, self.size)
        v = self.ap[:, a:a + n].bitcast(dt)
        if len(shape) == 2:
            v = v.rearrange("p (a b) -> p a b", a=shape[0])
        elif len(shape) == 3:
            v = v.rearrange("p (a b c) -> p a b c", a=shape[0], b=shape[1])
        return v


def _bf(a):
    return np.ascontiguousarray(a.astype(ml_dtypes.bfloat16))


def host_constants():
    C = {}
    C["ident_f"] = np.eye(128, dtype=np.float32)
    C["ones_f"] = np.ones((128, 128), np.float32)
    C["ones_b"] = _bf(np.ones((128, 128), np.float32))
    bd = np.zeros((128, 128), np.float32)
    bd[:64, :64] = 1
    bd[64:, 64:] = 1
    C["bd_b"] = _bf(bd)
    rot = np.zeros((128, 128), np.float32)
    for i in range(64):
        rot[2 * i + 1, 2 * i] = -1.0
        rot[2 * i, 2 * i + 1] = 1.0
    C["rot_b"] = _bf(rot)
    jj = np.zeros((128, 128), np.float32)
    for p in range(64):
        jj[p, 63 - p] = 1
        jj[64 + p, 64 + 63 - p] = 1
    C["jrev_b"] = _bf(jj)
    sel = np.zeros((32, 32 * 128), np.float32)
    for e in range(32):
        sel[e, e * 128:(e + 1) * 128] = 1.0 / 1.702
    C["sel_f"] = sel
    pos = np.arange(SEQ)
    row = (pos // 64).astype(np.float32)
    col = (pos % 64).astype(np.float32)
    inv_freq = (np.float32(10000.0) ** (-np.arange(0, 32, 2, dtype=np.float32) / np.float32(32))).astype(np.float32)
    ang = np.concatenate([row[:, None] * inv_freq, col[:, None] * inv_freq], -1).astype(np.float32)
    cosT = np.cos(ang).astype(np.float32).T
    sinT = np.sin(ang).astype(np.float32).T
    C["cosT"] = np.ascontiguousarray(np.tile(np.repeat(cosT, 2, axis=0), (2, 1)))
    C["sinT"] = np.ascontiguousarray(np.tile(np.repeat(sinT, 2, axis=0), (2, 1)))
    cv = np.zeros((128, 64), np.float32)
    for qc in range(64):
        c0 = min(max(qc - 8, 0), 48)
        cv[c0:c0 + 16, qc] = 1
        cv[64 + c0:64 + c0 + 16, qc] = 1
    C["cvalid_b"] = _bf(cv)
    Wp = np.zeros((128, 512), np.float32)
    for jx in range(16):
        i = jx % 8
        Wp[jx, i * 64:(i + 1) * 64] = -10000.0
        Wp[64 + jx, i * 64:(i + 1) * 64] = -10000.0
    C["na_w_b"] = _bf(Wp)
    tabs = []
    tab_index = {}
    for R in range(8):
        for m in na_chunks(R):
            key = ("int", m - 4 * R) if 1 <= R <= 6 else (R, m)
            if key in tab_index:
                continue
            U = np.zeros((128, 128), np.float32)
            for half in range(2):
                kr = 2 * m + half
                for i in range(8):
                    qr = 8 * R + i
                    r0 = min(max(qr - 4, 0), 56)
                    if not (r0 <= kr <= r0 + 7):
                        U[half * 8 + i, half * 64:(half + 1) * 64] = 1.0
                        U[64 + half * 8 + i, half * 64:(half + 1) * 64] = 1.0
            tab_index[key] = len(tabs)
            tabs.append(U)
    C["na_u_b"] = _bf(np.concatenate(tabs, axis=1))
    C["_na_tab_index"] = tab_index
    posf = np.arange(SEQ, dtype=np.float32)
    t = posf / np.float32(SEQ - 1)
    w = np.float32(2.0 * math.pi) * posf / np.float32(SEQ)
    f = np.linspace(1e-4, 15, 16, dtype=np.float32)
    wf = w[:, None] * f[None, :]
    z = np.concatenate([t[:, None], np.cos(wf), -np.sin(wf)], -1).astype(np.float32)
    C["hy_zT"] = np.ascontiguousarray(z.T)
    C["hy_negt"] = np.ascontiguousarray((-t).reshape(32, 128).T)
    max_decay = math.log(1e-2) / 0.3
    min_decay = math.log(1e-2) / 1.5
    deltas = np.abs(np.linspace(min_decay, max_decay, 768, dtype=np.float32)).astype(np.float32)
    C["hy_delta"] = np.ascontiguousarray(deltas.reshape(1, 768))
    n = 2 * SEQ
    tt = np.arange(SEQ, dtype=np.int64)
    ff = np.arange(SEQ, dtype=np.int64)
    idx = (np.outer(tt, ff) % n)
    ctab = np.cos(np.arange(n, dtype=np.float64) * (2.0 * math.pi / n)).astype(np.float32)
    stab = np.sin(np.arange(n, dtype=np.float64) * (2.0 * math.pi / n)).astype(np.float32)
    cosm = ctab[idx]
    sinm = stab[idx]
    fwd = np.empty((SEQ, 2 * SEQ), np.float32)
    fwd[:, :SEQ] = cosm
    fwd[:, SEQ:] = -sinm
    fwd[:, SEQ] = np.where(tt % 2 == 0, 1.0, -1.0)
    fw = fwd.reshape(32, 128, 64, 128).transpose(2, 1, 0, 3)
    C["dft_fwd"] = _bf(fw.reshape(64 * 128, 32 * 128))
    inv = np.empty((2 * SEQ, SEQ), np.float32)
    inv[:SEQ] = (2.0 / n) * cosm.T
    inv[SEQ:] = -(2.0 / n) * sinm.T
    inv[0] = 1.0 / n
    inv[SEQ] = np.where(tt % 2 == 0, 1.0, -1.0) / n
    iv = inv.reshape(64, 128, 32, 128).transpose(2, 1, 0, 3)
    C["dft_inv"] = _bf(iv.reshape(32 * 128, 64 * 128))
    return C


GQA_PAIRS = [(0, 3), (1, 4), (2, 5), (6, 9), (7, 10), (8, 11)]


def na_chunks(R):
    lo = min(max(8 * R - 4, 0), 56)
    hi = min(max(8 * R + 7 - 4, 0), 56) + 7
    return list(range(lo // 2, hi // 2 + 1))


class LazyIn:
    def __init__(self, bld, shapes):
        self.bld, self.shapes, self.d = bld, shapes, {}

    def __getitem__(self, k):
        if k not in self.d:
            name, shape, dt = self.shapes[k]
            self.d[k] = self.bld.nc.dram_tensor(name, list(shape), dt, kind="ExternalInput").ap()
            self.bld.used_inputs.append(name)
        return self.d[k]


class Builder:
    def __init__(self, consts, mode="full"):
        self.mode = mode
        self.used_inputs = []
        self.C = consts
        self.nc = bass.Bass("TRN2", target_bir_lowering=False)
        self.P = Prog(self.nc, same_engine_sync=CFG["ses"])
        self.es = ExitStack()
        self.uid = 0
        self.din = {}

    def inp(self, name, shape, dt=F32):
        t = self.nc.dram_tensor(name, list(shape), dt, kind="ExternalInput").ap()
        self.din[name] = t
        return t

    def scratch(self, name, shape, dt):
        return self.nc.dram_tensor(name, list(shape), dt, kind="Internal").ap()

    def psum(self, name, shape=(128, 512), dt=F32):
        return self.es.enter_context(self.nc.psum_tensor(name, list(shape), dt))[:, :]

    def key(self, base):
        self.uid += 1
        return f"{base}#{self.uid}"

    def mm(self, out, lhsT, rhs, start, stop, reads, writes):
        return self.P.op("pe", lambda e: e.matmul(out, lhsT=lhsT, rhs=rhs, start=start, stop=stop), reads, writes)

    def tp(self, out, in_, ident, reads, writes):
        return self.P.op("pe", lambda e: e.transpose(out, in_, ident), reads, writes)

    def act(self, out, in_, func, reads, writes, bias=None, scale=None):
        kw = {}
        if bias is not None:
            kw["bias"] = bias
        if scale is not None:
            kw["scale"] = scale
        return self.P.op("act", lambda e: e.activation(out=out, in_=in_, func=func, **kw), reads, writes)

    def tt(self, eng, out, in0, in1, op, reads, writes):
        return self.P.op(eng, lambda e: e.tensor_tensor(out=out, in0=in0, in1=in1, op=op), reads, writes)

    def ts(self, eng, out, in0, s1, s2, op0, op1, reads, writes):
        if op1 is None:
            return self.P.op(eng, lambda e: e.tensor_scalar(out=out, in0=in0, scalar1=s1, scalar2=None, op0=op0), reads, writes)
        return self.P.op(eng, lambda e: e.tensor_scalar(out=out, in0=in0, scalar1=s1, scalar2=s2, op0=op0, op1=op1), reads, writes)

    def stt(self, out, in0, scalar, in1, op0, op1, reads, writes):
        return self.P.op("dve", lambda e: e.scalar_tensor_tensor(out=out, in0=in0, scalar=scalar, in1=in1, op0=op0, op1=op1), reads, writes)

    def cp(self, eng, out, in_, reads, writes):
        if eng == "act":
            return self.P.op("act", lambda e: e.copy(out=out, in_=in_), reads, writes)
        return self.P.op(eng, lambda e: e.tensor_copy(out=out, in_=in_), reads, writes)

    def recip(self, out, in_, reads, writes):
        return self.P.op("dve", lambda e: e.reciprocal(out=out, in_=in_), reads, writes)

    def build(self):
        nc, P, C = self.nc, self.P, self.C
        es = self.es
        with es:
            self._build()
        return nc

    def _build(self):
        nc, P, C, es = self.nc, self.P, self.C, self.es
        mode = self.mode
        launch = mode != "full"
        nl = 1 if launch else DEPTH
        sh = {}
        def reg(k, shape, dt=F32, name=None):
            sh[k] = (name or k, shape, dt)
        reg("x", [NT, D]); reg("mem", [NB * 256, D])
        reg("w_in_a", [2 * D, 2560]); reg("rpb", [2 * 12, 24 * 31])
        reg("w_in_b", [D, 1536]); reg("q_norm_b", [64, 1]); reg("k_norm_b", [64, 1])
        reg("w_in_c", [D, 2560]); reg("conv_w_c", [3, 2304]); reg("conv_b_c", [1, 2304])
        reg("filt_w1", [33, 64]); reg("filt_b1", [64, 1]); reg("filt_w2", [64, 64]); reg("filt_b2", [64, 1])
        reg("filt_w3", [64, 64]); reg("filt_b3", [64, 1]); reg("filt_w4", [64, 3072]); reg("filt_b4", [1, 3072])
        reg("filt_freq", [64, 1]); reg("long_bias_c", [2, 768])
        reg("w_mem_kv", [DEPTH * D, 512]); reg("w_out", [DEPTH * D, D])
        for nm in ("ln1_g", "ln1_b", "ln2_g", "ln2_b"):
            reg(nm, [DEPTH * 8, 128])
        reg("router_w", [DEPTH * D, NE]); reg("router_b", [DEPTH, NE])
        reg("moe_w1", [nl * NE * D, 2 * DE]); reg("moe_b1", [DEPTH * NE, 2 * DE])
        reg("moe_w2", [nl * NE * DE, D]); reg("moe_b2", [DEPTH * NE, D])
        I = LazyIn(self, sh)
        csh = {}
        for k, v in C.items():
            if k.startswith("_"):
                continue
            csh[k] = ("c_" + k, v.shape, BF16 if v.dtype == ml_dtypes.bfloat16 else F32)
        cdt = LazyIn(self, csh)
        self.I = I
        self.cd = cdt
        self.out = None
        if not launch:
            self.out = nc.dram_tensor("out", [NT, D], F32, kind="ExternalOutput").ap()
        def xt(name, dt, kind):
            return nc.dram_tensor(name, [D, NT], dt, kind=kind).ap()
        k0 = {"full": "Internal", "pro": "ExternalOutput", "moe": "ExternalOutput"}.get(mode, "ExternalInput")
        k1 = {"full": "Internal", "moe": "ExternalInput", "pro": None}.get(mode, "ExternalOutput")
        self.X0f = xt("X0f", F32, k0)
        self.X0b = xt("X0b", BF16, k0)
        if k0 == "ExternalInput":
            self.used_inputs += ["X0f", "X0b"]
        if k1 is not None:
            self.X1f = xt("X1f", F32, k1)
            self.X1b = xt("X1b", BF16, k1)
            if k1 == "ExternalInput":
                self.used_inputs += ["X1f", "X1b"]
        nwl = DEPTH if mode == "full" else (1 if mode == "moe" else 0)
        self.WB1 = [self.scratch(f"WB1_{l}", [NE * 2 * 128, 8 * 1024], BF16) for l in range(nwl)]
        self.WB2 = [self.scratch(f"WB2_{l}", [NE * 2 * 128, 4 * 1024], BF16) for l in range(nwl)]
        self.CTs = self.scratch("CTs", [DEPTH * NE, NT], F32)
        self.dbg = None
        if CFG["dump"] is not None:
            self.dbg = nc.dram_tensor("dbg", [D, NT], F32, kind="ExternalOutput").ap()
        ARENA = 200 * 1024
        ar_t = es.enter_context(nc.sbuf_tensor("arena", [128, ARENA], U8))
        self.A = Arena(ar_t[:, :], ARENA)
        self.pb = [self.psum(f"pb{i}") for i in range(8)]
        sems = {k: es.enter_context(nc.semaphore(k)) for k in ("act", "dve", "pool", "pe")}
        sems["dma"] = {
            "sync": [es.enter_context(nc.semaphore(f"ds{i}")) for i in range(16)],
            "act": [es.enter_context(nc.semaphore(f"da{i}")) for i in range(8)],
            "pool": [es.enter_context(nc.semaphore(f"dp{i}")) for i in range(8)],
        }
        A = self.A
        self.ident_f = A.alloc([128], F32)
        self.ones_f = A.alloc([128], F32)
        self.ones_b = A.alloc([128], BF16)
        self.lnp = {nm: A.alloc([DEPTH * 8], F32) for nm in ("ln1_g", "ln1_b", "ln2_g", "ln2_b")}
        A.mark()
        P.dma(self.ident_f, cdt["ident_f"][:, :], writes=["ident_f"])
        P.dma(self.ones_f, cdt["ones_f"][:, :], writes=["ones_f"])
        P.dma(self.ones_b, cdt["ones_b"][:, :], writes=["ones_b"])
        if mode == "full":
            self.prologue()
            P.barrier()
            if CFG["moe"]:
                self.cast_moe_weights(CFG["layers"])
                P.barrier()
            for li in CFG["layers"]:
                self.layer(li)
        elif mode == "pro":
            self.prologue()
        elif mode in ("mixA", "mixB", "mixC"):
            self.prologue(ln_only=True)
            P.barrier()
            self.layer({"mixA": 0, "mixB": 1, "mixC": 2}[mode], mixer_only=True)
        elif mode == "moe":
            self.prologue(ln_only=True)
            P.barrier()
            self.cast_moe_weights([0])
            P.barrier()
            self.moe(0)
            P.barrier()
        fin = P.op("sync", None, extra_deps=list(P.dma_since))
        P.emit(sems)

    def load_T32(self, dst, src_rows, ncols, tag):
        P, A = self.P, self.A

    def prologue(self, ln_only=False):
        P, A, I = self.P, self.A, self.I
        A.reset()
        CFG["pro"] = "ln" if ln_only else ""
        stg = A.alloc([4, 128], F32)
        for i, nm in enumerate(("ln1_g", "ln1_b", "ln2_g", "ln2_b")):
            if CFG.get("pro", "") == "x":
                break
            P.dma(stg[0:32, i, :], I[nm][:, :], writes=[f"lnstg{i}"])
            ps = self.pb[i]
            self.tp(ps[:, 0:32], stg[0:32, i, :], self.ident_f[0:32, 0:32], reads=[f"lnstg{i}", "ident_f"], writes=[f"pb{i}"])
            self.cp("dve", self.lnp[nm], ps[:, 0:32], reads=[f"pb{i}"], writes=["lnp_" + nm])
        NBUF = 3
        xin = [A.alloc([D], F32) for _ in range(NBUF)]
        xo_f = [A.alloc([8, TT], F32) for _ in range(2)]
        xo_b = [A.alloc([8, TT], BF16) for _ in range(2)]
        nblk = NT // 128
        if CFG.get("pro", "") == "ln":
            nblk = -2
        for k in range(nblk + 2):
            if k < nblk:
                P.dma(xin[k % NBUF], I["x"][k * 128:(k + 1) * 128, :], writes=[f"xin{k % NBUF}"])
            j = k - 2
            if j >= 0:
                grp, blk = j // 4, j % 4
                s = grp % 2
                for g in range(2):
                    ps = self.pb[(j % 2) * 2 + g]
                    pk = f"pb{(j % 2) * 2 + g}"
                    for cc in range(4):
                        c = g * 4 + cc
                        self.tp(ps[:, cc * 128:(cc + 1) * 128], xin[j % NBUF][:, c * 128:(c + 1) * 128], self.ident_f, reads=[f"xin{j % NBUF}", "ident_f"], writes=[pk])
                    psv = ps.rearrange("p (c t) -> p c t", c=4)
                    self.cp("dve", xo_f[s][:, g * 4:(g + 1) * 4, blk * 128:(blk + 1) * 128], psv, reads=[pk], writes=[f"xof{s}"])
                    self.cp("act", xo_b[s][:, g * 4:(g + 1) * 4, blk * 128:(blk + 1) * 128], psv, reads=[pk], writes=[f"xob{s}"])
                if blk == 3:
                    self.fm_store(self.X0f, grp * TT, TT, xo_f[s], f"xof{s}")
                    self.fm_store(self.X0b, grp * TT, TT, xo_b[s], f"xob{s}")

    def fm_load(self, dst, dram, t0, n, wkeys, eng="sync"):
        rk = self.xkeys(dram.tensor.name, t0, n)
        for c in range(8):
            self.P.dma(dst[:, c, :], dram[c * 128:(c + 1) * 128, t0:t0 + n], reads=rk, writes=[wkeys[c]] if isinstance(wkeys, list) else [wkeys], eng=eng)

    def fm_store(self, dram, t0, n, src, rkeys, eng="act"):
        for c in range(8):
            wk = [f"{dram.tensor.name}@{k}_{c}" for k in range(t0 // 128, (t0 + n) // 128)]
            self.P.dma(dram[c * 128:(c + 1) * 128, t0:t0 + n], src[:, c, :], reads=[rkeys[c]] if isinstance(rkeys, list) else [rkeys], writes=wk, eng=eng)

    def xk(self, name, tok):
        return f"{name}@{tok // 128}"

    def xkeys(self, name, t0, n):
        return [f"{name}@{k}_{c}" for k in range(t0 // 128, (t0 + n) // 128) for c in range(8)]

    def cast_moe_weights(self, layers):
        P, A, I = self.P, self.A, self.I
        A.reset()
        NBUF = 4
        stg = [A.alloc([2048], F32) for _ in range(NBUF)]
        stb = [A.alloc([2048], BF16) for _ in range(NBUF)]
        jobs = []
        for li in layers:
            for e in range(CFG["n_experts"]):
                le = li * NE + e
                r1 = le * D
                for c in range(8):
                    jobs.append(("w1", I["moe_w1"][r1 + c * 128: r1 + (c + 1) * 128, :], le, c))
                r2 = le * DE
                for c2 in range(4):
                    src = I["moe_w2"][r2 + c2 * 256: r2 + (c2 + 1) * 256, :].rearrange("(a p) n -> p a n", p=128)
                    jobs.append(("w2", src, le, c2))
        engs = ["pool", "act", "dve", "pool"]
        n = len(jobs)
        for k in range(n + 2):
            if k < n:
                s = k % NBUF
                kind, src, le, c = jobs[k]
                d = stg[s] if kind == "w1" else stg[s].rearrange("p (a n) -> p a n", a=2)
                P.dma(d, src, writes=[f"cstg{s}"])
            j = k - 2
            if j >= 0:
                s = j % NBUF
                kind, src, le, c = jobs[j]
                self.cp(engs[j % 4], stb[s], stg[s], reads=[f"cstg{s}"], writes=[f"cstb{s}"])
                q = "act" if j % 2 else "pool"
                lw, ew = le // NE, le % NE
                if kind == "w1":
                    v = stb[s].rearrange("p (g h n) -> p g h n", g=2, h=2)
                    for hf in range(2):
                        row0 = (ew * 2 + hf) * 128
                        dst = self.WB1[lw][row0:row0 + 128, c * 1024:(c + 1) * 1024].rearrange("p (g n) -> p g n", g=2)
                        P.dma(dst, v[:, :, hf, :], reads=[f"cstb{s}"], writes=[self.key("wb")], eng=q)
                else:
                    hf = c // 2
                    jj0 = (2 * c) % 4
                    row0 = (ew * 2 + hf) * 128
                    dst = self.WB2[lw][row0:row0 + 128, jj0 * 1024:(jj0 + 2) * 1024].rearrange("p (a n) -> p a n", a=2)
                    P.dma(dst, stb[s].rearrange("p (a n) -> p a n", a=2), reads=[f"cstb{s}"], writes=[self.key("wb")], eng=q)

    def load_w_bf16(self, dst, src, ncols, tag, col0=0, rowmap=None):
        P = self.P
        stg = self._wstg
        k = 0
        step = 1024
        for c in range(8):
            pieces = [(0, c * 128, 128)] if rowmap is None else rowmap[c]
            for n0 in range(0, ncols, step):
                n1 = min(ncols, n0 + step)
                s = self._wstg_i % 2
                self._wstg_i += 1
                for (p0, r0, n) in pieces:
                    P.dma(stg[s][p0:p0 + n, 0:n1 - n0], src[r0:r0 + n, col0 + n0: col0 + n1], writes=[f"wstg{s}"])
                self.cp(["pool", "act"][k % 2], dst[:, c, n0:n1], stg[s][:, 0:n1 - n0], reads=[f"wstg{s}"], writes=[tag])
                k += 1

    def layernorm(self, z, gname, li, outb, zks, obk, tmp):
        P = self.P
        rot, mean, msq, var, rstd = tmp["rot"], tmp["mean"], tmp["msq"], tmp["var"], tmp["rstd"]
        ps_s, ps_q = self.pb[6], self.pb[7]
        for c in range(8):
            r = rot[c % 2]
            self.act(r, z[:, c, :], AF.Square, reads=[zks[c]], writes=[f"ln_rot{c % 2}"])
            self.mm(ps_s, self.ones_f, z[:, c, :], c == 0, c == 7, reads=["ones_f", zks[c]], writes=["pb6"])
            self.mm(ps_q, self.ones_f, r, c == 0, c == 7, reads=["ones_f", f"ln_rot{c % 2}"], writes=["pb7"])
        self.ts("dve", mean, ps_s, 1.0 / D, None, ALU.mult, None, reads=["pb6"], writes=["ln_mean"])
        self.tt("pool", msq, mean, mean, ALU.mult, reads=["ln_mean"], writes=["ln_msq"])
        self.stt(var, ps_q, 1.0 / D, msq, ALU.mult, ALU.subtract, reads=["pb7", "ln_msq"], writes=["ln_var"])
        self.act(var, var, AF.Sqrt, reads=["ln_var", "ln_eps"], writes=["ln_var"], bias=self.eps_t[:, 0:1])
        self.recip(rstd, var, reads=["ln_var"], writes=["ln_rstd"])
        g = self.lnp[gname + "_g"]
        bb = self.lnp[gname + "_b"]
        for c in range(8):
            zc = z[:, c, :]
            self.tt("dve", zc, zc, mean, ALU.subtract, reads=[zks[c], "ln_mean"], writes=[zks[c]])
            self.tt("pool", zc, zc, rstd, ALU.mult, reads=[zks[c], "ln_rstd"], writes=[zks[c]])
            self.act(zc, zc, AF.Identity, reads=[zks[c], "lnp_" + gname + "_g", "lnp_" + gname + "_b"], writes=[zks[c]],
                     bias=bb[:, li * 8 + c: li * 8 + c + 1], scale=g[:, li * 8 + c: li * 8 + c + 1])
            self.cp("pool", outb[:, c, :], zc, reads=[zks[c]], writes=[obk[c]])

    def ln_tmp(self):
        A = self.A
        return {"rot": [A.alloc([TT], F32) for _ in range(2)], "mean": A.alloc([TT], F32), "msq": A.alloc([TT], F32),
                "var": A.alloc([TT], F32), "rstd": A.alloc([TT], F32)}

    def layer(self, li, mixer_only=False):
        kind = li % 3
        j = li // 3
        P, A, I = self.P, self.A, self.I
        A.reset()
        self.eps_t = A.alloc([1], F32)
        self.eps6 = A.alloc([1], F32)
        P.op("pool", lambda e: e.memset(self.eps_t, LN_EPS), writes=["ln_eps"])
        P.op("pool", lambda e: e.memset(self.eps6, 1e-6), writes=["ln_eps6"])
        kmT = A.alloc([NB, 2, 256], BF16)
        vm = A.alloc([NB, 2, 256], BF16)
        save = A.off
        self._wstg = [A.alloc([1024], F32) for _ in range(2)]
        self._wstg_i = 0
        w_kv = A.alloc([8, 512], BF16)
        memT = A.alloc([NB, 8, 256], BF16)
        mst = A.alloc([2, D], F32)
        self.load_w_bf16(w_kv, I["w_mem_kv"][li * D:(li + 1) * D, :], 512, "w_kv")
        for b in range(NB):
            for mc in range(2):
                P.dma(mst[:, mc, :], I["mem"][b * 256 + mc * 128: b * 256 + (mc + 1) * 128, :], writes=[f"mst{mc}"])
                for g in range(2):
                    ps = self.pb[4 + g]
                    for cc in range(4):
                        c = g * 4 + cc
                        self.tp(ps[:, cc * 128:(cc + 1) * 128], mst[:, mc, c * 128:(c + 1) * 128], self.ident_f, reads=[f"mst{mc}", "ident_f"], writes=[f"pb{4+g}"])
                    self.cp("act" if g else "dve", memT[:, b, g * 4:(g + 1) * 4, mc * 128:(mc + 1) * 128],
                            ps.rearrange("p (c t) -> p c t", c=4), reads=[f"pb{4+g}"], writes=["memT"])
        for b in range(NB):
            for ch in range(2):
                ps = self.pb[ch]
                for c in range(8):
                    self.mm(ps[:, 0:256], w_kv[:, c, ch * 128:(ch + 1) * 128], memT[:, b, c, :], c == 0, c == 7, reads=["w_kv", "memT"], writes=[f"pb{ch}"])
                self.cp("act", kmT[:, b, ch, :], ps[:, 0:256], reads=[f"pb{ch}"], writes=["kmT"])
            for mc in range(2):
                ps = self.pb[2 + mc]
                for c in range(8):
                    self.mm(ps[:, 0:256], memT[:, b, c, mc * 128:(mc + 1) * 128], w_kv[:, c, 256:512], c == 0, c == 7, reads=["w_kv", "memT"], writes=[f"pb{2+mc}"])
                self.cp("dve", vm[:, b, mc, :], ps[:, 0:256], reads=[f"pb{2+mc}"], writes=["vm"])
        P.barrier()
        A.off = save
        self.kmT, self.vm = kmT, vm
        if kind == 2:
            self.mixer_hyena(li, j)
        else:
            if kind == 0:
                win_src, win_cols = I["w_in_a"][j * D:(j + 1) * D, :], 2560
            else:
                win_src, win_cols = I["w_in_b"], 1536
            w_in = A.alloc([8, win_cols], BF16)
            w_out = A.alloc([8, D], BF16)
            save = A.off
            self._wstg = [A.alloc([1024], F32) for _ in range(2)]
            self._wstg_i = 0
            self.load_w_bf16(w_in, win_src, win_cols, "w_in")
            rowmap = None
            if kind == 1:
                rowmap = []
                for ch in range(8):
                    if ch < 6:
                        a_, b_ = GQA_PAIRS[ch]
                        rowmap.append([(0, a_ * 64, 64), (64, b_ * 64, 64)])
                    else:
                        rowmap.append([(0, ch * 128, 128)])
            self.load_w_bf16(w_out, I["w_out"][li * D:(li + 1) * D, :], D, "w_out", rowmap=rowmap)
            P.barrier()
            A.off = save
            self.w_in, self.w_out = w_in, w_out
            self.qm_col0 = win_cols - 256
            if kind == 0:
                self.mixer_na(li, j)
            else:
                self.mixer_gqa(li, j)
        P.barrier()
        if CFG["moe"] and not mixer_only:
            self.moe(li)
            P.barrier()

    def attn_tail_bufs(self):
        A = self.A
        T = {}
        T["x0b"] = [A.alloc([8, TT], BF16) for _ in range(2)]
        T["z"] = A.alloc([8, TT], F32)
        T["cat"] = A.alloc([8, TT], BF16)
        T["qm"] = A.alloc([2, TT], BF16)
        T["pT"] = [A.alloc([TT], BF16) for _ in range(3)]
        T["rden"] = [A.alloc([TT], F32) for _ in range(2)]
        T["ln"] = self.ln_tmp()
        self.pT_i = 0
        self.od_i = 0
        return T

    def load_x0(self, T, tg, slot):
        P = self.P
        self.fm_load(T["x0b"][slot], self.X0b, tg, TT, f"x0b{slot}")

    def proj_fm(self, dst, xb, xk, w, col0, nch, dk, evac="act", ps_ids=(0, 1)):
        for ch in range(nch):
            pid = ps_ids[ch % len(ps_ids)]
            ps = self.pb[pid]
            for c in range(8):
                self.mm(ps, w[:, c, col0 + ch * 128: col0 + (ch + 1) * 128], xb[:, c, :], c == 0, c == 7, reads=["w_in", xk], writes=[f"pb{pid}"])
            self.cp(evac if ch % 2 == 0 else ("dve" if evac == "act" else "act"), dst[:, ch, :], ps, reads=[f"pb{pid}"], writes=[dk])

    def attend(self, T, q_ap, qk, kv_list, out_ap, outk, half, mask_fn=None, post_fn=None):
        P = self.P
        oi = self.od_i % 2
        self.od_i += 1
        po, pd = self.pb[4 + oi * 2], self.pb[5 + oi * 2]
        pok, pdk = f"pb{4 + oi * 2}", f"pb{5 + oi * 2}"
        n = len(kv_list)
        for idx, (kT, kkeys, v, vkeys, extra) in enumerate(kv_list):
            si = self.pT_i % 3
            self.pT_i += 1
            ps = self.pb[si]
            psk = f"pb{si}"
            pT = T["pT"][si]
            pTk = f"pT{si}"
            if mask_fn is None:
                self.mm(ps, kT, q_ap, True, True, reads=list(kkeys) + [qk], writes=[psk])
            else:
                self.mm(ps, kT, q_ap, True, False, reads=list(kkeys) + [qk], writes=[psk])
                mask_fn(ps, psk, extra)
            if post_fn is None:
                self.act(pT, ps, AF.Exp, reads=[psk], writes=[pTk], scale=0.125)
            else:
                post_fn(pT, pTk, ps, psk, extra, si)
            self.mm(po, v, pT, idx == 0, idx == n - 1, reads=list(vkeys) + [pTk], writes=[pok])
            self.mm(pd, self.ones_b, pT, idx == 0, idx == n - 1, reads=["ones_b", pTk], writes=[pdk])
        r = T["rden"][oi]
        rk = f"rden{oi}"
        sl = slice(half * 64, (half + 1) * 64)
        self.recip(r[sl, :], pd[sl, :], reads=[pdk], writes=[rk])
        self.tt("dve", out_ap, po[sl, :], r[sl, :], ALU.mult, reads=[pok, rk], writes=[outk])

    def tail(self, T, li, b, tg, slot):
        P = self.P
        xb = T["x0b"][slot]
        xk = f"x0b{slot}"
        catk = [f"cat{c}" for c in range(8)]
        zk = [f"z{c}" for c in range(8)]
        self.proj_fm(T["qm"], xb, xk, self.w_in, self.qm_col0, 2, "qm", ps_ids=(6, 7))
        for hm in range(4):
            ch, half = hm // 2, hm % 2
            sl = slice(half * 64, (half + 1) * 64)
            kv = []
            for mc in range(2):
                kv.append((self.kmT[sl, b, ch, mc * 128:(mc + 1) * 128], ["kmT"], self.vm[:, b, mc, ch * 128:(ch + 1) * 128], ["vm"], None))
            self.attend(T, T["qm"][sl, ch, :], "qm", kv, T["cat"][sl, 6 + ch, :], catk[6 + ch], half)
        self.fm_load(T["z"], self.X0f, tg, TT, zk)
        for fo in range(8):
            pid = fo % 2
            ps = self.pb[pid]
            for cc in range(8):
                self.mm(ps, self.w_out[:, cc, fo * 128:(fo + 1) * 128], T["cat"][:, cc, :], cc == 0, cc == 7, reads=["w_out", catk[cc]], writes=[f"pb{pid}"])
            zc = T["z"][:, fo, :]
            self.stt(zc, zc, ALPHA, ps, ALU.mult, ALU.add, reads=[zk[fo], f"pb{pid}"], writes=[zk[fo]])
        self.layernorm(T["z"], "ln1", li, T["cat"], zk, catk, T["ln"])
        self.fm_store(self.X1f, tg, TT, T["z"], zk)
        self.fm_store(self.X1b, tg, TT, T["cat"], catk)

    def mixer_na(self, li, j):
        P, A, I, cd = self.P, self.A, self.I, self.cd
        tabidx = self.C["_na_tab_index"]
        ntab = len(tabidx)
        u_b = A.alloc([ntab, 128], BF16)
        w_b = A.alloc([512], BF16)
        P.dma(u_b, cd["na_u_b"].rearrange("p (a b) -> p a b", b=128), writes=["na_u"])
        P.dma(w_b, cd["na_w_b"][:, :], writes=["na_w"])
        Gd = self.scratch(f"na_G{li}", [12 * 128, 1472], BF16)
        save = A.off
        cv = A.alloc([64], BF16)
        jrev = A.alloc([128], BF16)
        P.dma(cv, cd["cvalid_b"][:, :], writes=["na_cv"])
        P.dma(jrev, cd["jrev_b"][:, :], writes=["jrev"])
        rp = A.alloc([24 * 31], F32)
        P.dma(rp[0:12, :], I["rpb"][j * 12:(j + 1) * 12, :], writes=["rp"])
        self.act(rp[0:12, :], rp[0:12, :], AF.Exp, reads=["rp"], writes=["rp"])
        tsrc = self.scratch(f"na_tsrc{li}", [1, 9216], F32)
        zt = A.alloc([160], F32)
        P.op("pool", lambda e: e.memset(zt, 0.0), writes=["zt"])
        P.dma(tsrc[0:1, 0:128], zt[0:1, 0:128], reads=["zt"], writes=["tsrc"])
        P.dma(tsrc[0:1, 128 + 8928:9216], zt[0:1, 0:9216 - 128 - 8928], reads=["zt"], writes=["tsrc"])
        P.dma(tsrc[0:1, 128:128 + 8928].rearrange("o (h n) -> (o h) n", h=12), rp[0:12, :], reads=["rp"], writes=["tsrc"])
        gt32 = [A.alloc([23, 64], F32) for _ in range(2)]
        gtb = [A.alloc([23, 64], BF16) for _ in range(2)]
        Gs = [A.alloc([23, 64], BF16) for _ in range(2)]
        for h in range(12):
            s = h % 2
            for half in range(2):
                for (sa, sb) in ((0, 6), (6, 12), (12, 18), (18, 23)):
                    src = bass.AP(tsrc.tensor, 128 + h * 744 + (1 - half + sa) * 31 - 48, [[1, 64], [31, sb - sa], [1, 64]])
                    P.dma(gt32[s][half * 64:(half + 1) * 64, sa:sb, :], src, reads=["tsrc"], writes=[f"gt32{s}"])
            self.cp("act", gtb[s], gt32[s], reads=[f"gt32{s}"], writes=[f"gtb{s}"])
            gflat = gtb[s].rearrange("p a b -> p (a b)")
            for k in range(3):
                n0, n1 = k * 512, min(1472, (k + 1) * 512)
                ps = self.pb[k]
                self.mm(ps[:, 0:n1 - n0], jrev, gflat[:, n0:n1], True, True, reads=["jrev", f"gtb{s}"], writes=[f"pb{k}"])
                nq = (n1 - n0) // 64
                self.tt("dve", Gs[s][:, n0 // 64: n0 // 64 + nq, :], ps[:, 0:n1 - n0].rearrange("p (a b) -> p a b", b=64),
                        cv.unsqueeze(1).to_broadcast([128, nq, 64]), ALU.mult, reads=[f"pb{k}", "na_cv"], writes=[f"Gs{s}"])
            P.dma(Gd[h * 128:(h + 1) * 128, :], Gs[s].rearrange("p a b -> p (a b)"), reads=[f"Gs{s}"], writes=["Gd"], eng="act")
        P.barrier()
        A.off = save
        if CFG.get("stop") == "gbuild":
            return
        G = [A.alloc([23, 64], BF16) for _ in range(2)]
        kT = A.alloc([6, 3 * TT], BF16)
        V = A.alloc([12, 768], BF16)
        T = self.attn_tail_bufs()
        q = A.alloc([6, TT], BF16)
        ex = [A.alloc([TT], BF16) for _ in range(3)]
        catk = [f"cat{c}" for c in range(8)]

        def produce(b, t):
            tg = b * SEQ + t * TT
            slot = t % 2
            ks = t % 3
            self.load_x0(T, tg, slot)
            xb, xk = T["x0b"][slot], f"x0b{slot}"
            self.proj_fm(kT[:, :, ks * TT:(ks + 1) * TT], xb, xk, self.w_in, 768, 6, f"kT_s{ks}")
            for blk in range(4):
                vi = ks * 4 + blk
                for (n0, n1, pid) in ((0, 512, 2), (512, 768, 3)):
                    ps = self.pb[pid]
                    for c in range(8):
                        self.mm(ps[:, 0:n1 - n0], xb[:, c, blk * 128:(blk + 1) * 128], self.w_in[:, c, 1536 + n0:1536 + n1], c == 0, c == 7, reads=["w_in", xk], writes=[f"pb{pid}"])
                    self.cp("dve" if pid == 2 else "act", V[:, vi, n0:n1], ps[:, 0:n1 - n0], reads=[f"pb{pid}"], writes=[f"V_s{ks}"])

        gi = 0
        for b in range(NB):
            produce(b, 0)
            produce(b, 1)
            for R in range(8):
                if R >= 1 and R + 1 <= 7:
                    produce(b, R + 1)
                tg = b * SEQ + R * TT
                slot = R % 2
                xb, xk = T["x0b"][slot], f"x0b{slot}"
                self.proj_fm(q, xb, xk, self.w_in, 0, 6, "q", ps_ids=(6, 7))
                chunks = na_chunks(R)
                for h in range(12):
                    ch, half = h // 2, h % 2
                    sl = slice(half * 64, (half + 1) * 64)
                    gs = gi % 2
                    gi += 1
                    P.dma(G[gs].rearrange("p a b -> p (a b)"), Gd[h * 128:(h + 1) * 128, :], writes=[f"G{gs}"])
                    kv = []
                    for m in chunks:
                        key = ("int", m - 4 * R) if 1 <= R <= 6 else (R, m)
                        s0 = 8 * R - 2 * m + 7 + 4
                        ks = (m // 4) % 3
                        col = ks * TT + (m % 4) * 128
                        kv.append((kT[sl, ch, col:col + 128], [f"kT_s{ks}"], V[:, ks * 4 + m % 4, ch * 128:(ch + 1) * 128], [f"V_s{ks}"], (tabidx[key], s0, gs)))

                    def mask_fn(ps, psk, extra, sl=sl):
                        ti = extra[0]
                        self.mm(ps, u_b[sl, ti, :], w_b[sl, :], False, True, reads=["na_u", "na_w"], writes=[psk])

                    def post_fn(pT, pTk, ps, psk, extra, si):
                        ti, s0, gs_ = extra
                        self.act(ex[si], ps, AF.Exp, reads=[psk], writes=[f"ex{si}"], scale=0.125)
                        self.tt("pool" if (self.pT_i % 2) else "dve", pT, ex[si], G[gs_][:, s0:s0 + 8, :].rearrange("p a b -> p (a b)"), ALU.mult, reads=[f"ex{si}", f"G{gs_}"], writes=[pTk])

                    self.attend(T, q[sl, ch, :], "q", kv, T["cat"][sl, ch, :], catk[ch], half, mask_fn=mask_fn, post_fn=post_fn)
                self.tail(T, li, b, tg, slot)

    def mixer_gqa(self, li, j):
        P, A, I, cd = self.P, self.A, self.I, self.cd
        bd = A.alloc([128], BF16)
        rot = A.alloc([128], BF16)
        gq = A.alloc([1], F32)
        gk = A.alloc([1], F32)
        P.dma(bd, cd["bd_b"][:, :], writes=["bd"])
        P.dma(rot, cd["rot_b"][:, :], writes=["rot"])
        for hf in range(2):
            P.dma(gq[hf * 64:(hf + 1) * 64, :], I["q_norm_b"][:, :], writes=["gq"])
            P.dma(gk[hf * 64:(hf + 1) * 64, :], I["k_norm_b"][:, :], writes=["gk"])
        kT = A.alloc([2, SEQ], BF16)
        V = A.alloc([32, 256], BF16)
        T = self.attn_tail_bufs()
        q = A.alloc([6, TT], BF16)
        cs = [A.alloc([2, TT], F32) for _ in range(2)]
        nb = {"qf": A.alloc([TT], F32), "sq": A.alloc([TT], BF16), "sd": A.alloc([TT], F32), "qn": A.alloc([TT], BF16),
              "t2": A.alloc([TT], F32)}
        catk = [f"cat{c}" for c in range(8)]

        def norm_rope(ps, psk, g_ap, gkey, dst, dk, cst, cstk):
            self.cp("act", nb["qf"], ps, reads=[psk], writes=["nb_qf"])
            self.act(nb["sq"], ps, AF.Square, reads=[psk], writes=["nb_sq"])
            p2 = self.pb[2]
            self.mm(p2, bd, nb["sq"], True, True, reads=["bd", "nb_sq"], writes=["pb2"])
            self.act(nb["sd"], p2, AF.Sqrt, reads=["pb2", "ln_eps6"], writes=["nb_sd"], bias=self.eps6[:, 0:1], scale=1.0 / 64)
            self.recip(nb["sd"], nb["sd"], reads=["nb_sd"], writes=["nb_sd"])
            self.stt(nb["qn"], nb["qf"], g_ap[:, 0:1], nb["sd"], ALU.mult, ALU.mult, reads=["nb_qf", "nb_sd", gkey], writes=["nb_qn"])
            p3 = self.pb[3]
            self.mm(p3, rot, nb["qn"], True, True, reads=["rot", "nb_qn"], writes=["pb3"])
            self.tt("pool", nb["qf"], nb["qn"], cst[:, 0, :], ALU.mult, reads=["nb_qn", cstk], writes=["nb_qf"])
            self.tt("dve", nb["t2"], p3, cst[:, 1, :], ALU.mult, reads=["pb3", cstk], writes=["nb_t2"])
            self.tt("pool", dst, nb["qf"], nb["t2"], ALU.add, reads=["nb_qf", "nb_t2"], writes=[dk])

        def load_cs(t, slot):
            P.dma(cs[slot][:, 0, :], cd["cosT"][:, t * TT:(t + 1) * TT], writes=[f"cs{slot}"])
            P.dma(cs[slot][:, 1, :], cd["sinT"][:, t * TT:(t + 1) * TT], writes=[f"cs{slot}"])

        for b in range(NB):
            for t in range(8):
                tg = b * SEQ + t * TT
                slot = t % 2
                self.load_x0(T, tg, slot)
                load_cs(t, slot)
                xb, xk = T["x0b"][slot], f"x0b{slot}"
                for ch in range(2):
                    ps = self.pb[ch]
                    for c in range(8):
                        self.mm(ps, self.w_in[:, c, 768 + ch * 128: 768 + (ch + 1) * 128], xb[:, c, :], c == 0, c == 7, reads=["w_in", xk], writes=[f"pb{ch}"])
                    norm_rope(ps, f"pb{ch}", gk, "gk", kT[:, ch, t * TT:(t + 1) * TT], f"kT{t}", cs[slot], f"cs{slot}")
                for blk in range(4):
                    m = t * 4 + blk
                    ps = self.pb[4 + blk % 2]
                    pk = f"pb{4 + blk % 2}"
                    for c in range(8):
                        self.mm(ps[:, 0:256], xb[:, c, blk * 128:(blk + 1) * 128], self.w_in[:, c, 1024:1280], c == 0, c == 7, reads=["w_in", xk], writes=[pk])
                    self.cp("dve" if blk % 2 else "act", V[:, m, :], ps[:, 0:256], reads=[pk], writes=[f"V{t}"])
            for t in range(8):
                tg = b * SEQ + t * TT
                slot = t % 2
                self.load_x0(T, tg, slot)
                load_cs(t, slot)
                xb, xk = T["x0b"][slot], f"x0b{slot}"
                for ch in range(6):
                    ps = self.pb[ch % 2]
                    for hf in range(2):
                        hh = GQA_PAIRS[ch][hf]
                        for c in range(8):
                            self.mm(ps[hf * 64:(hf + 1) * 64, :], self.w_in[:, c, hh * 64:(hh + 1) * 64], xb[:, c, :], c == 0, c == 7, reads=["w_in", xk], writes=[f"pb{ch % 2}"])
                    norm_rope(ps, f"pb{ch % 2}", gq, "gq", q[:, ch, :], f"q{ch}", cs[slot], f"cs{slot}")
                for ch in range(6):
                    for hf in range(2):
                        h = GQA_PAIRS[ch][hf]
                        kvh = h // 3
                        assert kvh % 2 == hf
                        sl = slice(hf * 64, (hf + 1) * 64)
                        kv = []
                        for m in range(32):
                            kv.append((kT[sl, kvh // 2, m * 128:(m + 1) * 128], [f"kT{m // 4}"], V[:, m, (kvh // 2) * 128:(kvh // 2 + 1) * 128], [f"V{m // 4}"], None))
                        self.attend(T, q[sl, ch, :], f"q{ch}", kv, T["cat"][sl, ch, :], catk[ch], hf)
                self.tail(T, li, b, tg, slot)

    def mixer_hyena(self, li, j):
        P, A, I, cd = self.P, self.A, self.I, self.cd
        base = A.off
        GA = [self.scratch(f"hyGA{o}", [SEQ, 768], F32) for o in range(2)]
        GB = [self.scratch(f"hyGB{o}", [SEQ, 768], F32) for o in range(2)]
        TM = [self.scratch(f"hyTM{k}", [NT, 768], BF16) for k in range(3)]
        mixT = self.scratch("hymixT", [768, NT], BF16)
        ident_b = A.alloc([128], BF16)
        self.cp("act", ident_b, self.ident_f, reads=["ident_f"], writes=["ident_b"])
        base = A.off
        TWO_PI = 2.0 * math.pi
        MAGIC = 12582912.0
        h3T = A.alloc([SEQ], F32)
        sv = A.off
        zT = A.alloc([SEQ], F32)
        hT1 = A.alloc([SEQ], F32)
        w1 = A.alloc([64], F32)
        w2 = A.alloc([64], F32)
        w3 = A.alloc([64], F32)
        fb = A.alloc([8], F32)
        P.dma(zT[0:33, :], cd["hy_zT"][:, :], writes=["zT"])
        P.dma(w1[0:33, :], I["filt_w1"][:, :], writes=["fw1"])
        P.dma(w2[0:64, :], I["filt_w2"][:, :], writes=["fw2"])
        P.dma(w3[0:64, :], I["filt_w3"][:, :], writes=["fw3"])
        for i, nm in enumerate(("filt_b1", "filt_b2", "filt_b3", "filt_freq")):
            P.dma(fb[0:64, i:i + 1], I[nm][:, :], writes=["fb"])
        tmp = [A.alloc([TT], F32) for _ in range(2)]
        kk = [A.alloc([TT], F32) for _ in range(2)]
        chain = [(zT, "zT", 33, w1, "fw1", 0, h3T, "h3T"), (h3T, "h3T", 64, w2, "fw2", 1, hT1, "hT1"), (hT1, "hT1", 64, w3, "fw3", 2, h3T, "h3T")]
        for (src, sk, K_, w_, wk, bi, dst, dk) in chain:
            for t in range(8):
                s = t % 2
                ps = self.pb[s]
                self.mm(ps[0:64, :], w_[0:K_, 0:64], src[0:K_, t * TT:(t + 1) * TT], True, True, reads=[wk, f"{sk}{t}"], writes=[f"pb{s}"])
                self.ts("dve", tmp[s][0:64, :], ps[0:64, :], fb[0:64, bi:bi + 1], fb[0:64, 3:4], ALU.add, ALU.mult, reads=[f"pb{s}", "fb"], writes=[f"htmp{s}"])
                self.ts("dve", kk[s][0:64, :], tmp[s][0:64, :], 1.0 / TWO_PI, MAGIC, ALU.mult, ALU.add, reads=[f"htmp{s}"], writes=[f"hkk{s}"])
                self.ts("dve", kk[s][0:64, :], kk[s][0:64, :], MAGIC, -TWO_PI, ALU.subtract, ALU.mult, reads=[f"hkk{s}"], writes=[f"hkk{s}"])
                self.tt("dve", tmp[s][0:64, :], tmp[s][0:64, :], kk[s][0:64, :], ALU.add, reads=[f"htmp{s}", f"hkk{s}"], writes=[f"htmp{s}"])
                self.act(dst[0:64, t * TT:(t + 1) * TT], tmp[s][0:64, :], AF.Sin, reads=[f"htmp{s}"], writes=[f"{dk}{t}"])
        P.barrier()
        A.off = sv
        w4 = A.alloc([3072], F32)
        b4bc = A.alloc([3072], F32)
        dbc = A.alloc([768], F32)
        negt = A.alloc([32], F32)
        P.dma(w4[0:64, :], I["filt_w4"][:, :], writes=["fw4"])
        P.dma(b4bc, I["filt_b4"][0:1, :].partition_broadcast(128), writes=["b4bc"])
        P.dma(dbc, cd["hy_delta"][0:1, :].partition_broadcast(128), writes=["dbc"])
        P.dma(negt, cd["hy_negt"][:, :], writes=["negt"])
        hdb = [A.alloc([32, TT], BF16) for _ in range(2)]
        hfl = [A.alloc([TT], F32) for _ in range(2)]
        dct = [A.alloc([TT], F32) for _ in range(2)]
        abt = [A.alloc([TT], F32) for _ in range(2)]
        ft = [A.alloc([2, 32, 128], BF16) for _ in range(2)]
        s0t = A.alloc([TT], F32)
        s1t = A.alloc([TT], F32)
        gre = [A.alloc([TT], F32) for _ in range(2)]
        gim = [A.alloc([TT], F32) for _ in range(2)]
        rnt = A.alloc([TT], F32)
        fi = 0
        for o in range(2):
            for (c0, c1) in ((0, 512), (512, 768)):
                w = c1 - c0
                for d in range(2):
                    q = d * 2 + o
                    pa = self.pb[4 + d]
                    for tc in range(32):
                        s = tc % 2
                        ps = self.pb[s]
                        self.mm(ps[:, 0:w], h3T[0:64, tc * 128:(tc + 1) * 128], w4[0:64, q * 768 + c0: q * 768 + c1], True, True, reads=["fw4"], writes=[f"pb{s}"])
                        self.act(dct[s][:, 0:w], dbc[:, c0:c1], AF.Exp, reads=["dbc", "negt"], writes=[f"dct{s}"], scale=negt[:, tc:tc + 1])
                        self.tt("dve", hfl[s][:, 0:w], ps[:, 0:w], b4bc[:, q * 768 + c0: q * 768 + c1], ALU.add, reads=[f"pb{s}", "b4bc"], writes=[f"hfl{s}"])
                        self.tt("pool", hfl[s][:, 0:w], hfl[s][:, 0:w], dct[s][:, 0:w], ALU.mult, reads=[f"hfl{s}", f"dct{s}"], writes=[f"hfl{s}"])
                        self.act(abt[s][:, 0:w], hfl[s][:, 0:w], AF.Abs, reads=[f"hfl{s}"], writes=[f"abt{s}"])
                        self.mm(pa[:, 0:w], self.ones_f, abt[s][:, 0:w], tc == 0, tc == 31, reads=["ones_f", f"abt{s}"], writes=[f"pb{4 + d}"])
                        if d == 1 and tc == 0:
                            P.op("pool", lambda e, s=s, w=w: e.memset(hfl[s][0:1, 0:w], 0.0), reads=[f"abt{s}"], writes=[f"hfl{s}"])
                        self.cp("pool" if tc % 2 else "dve", hdb[d][:, tc, 0:w], hfl[s][:, 0:w], reads=[f"hfl{s}"], writes=[f"hdb{d}_{tc}"])
                self.cp("act", s0t[:, 0:w], self.pb[4][:, 0:w], reads=["pb4"], writes=["s0t"])
                self.tt("dve", rnt[:, 0:w], self.pb[5][:, 0:w], s0t[:, 0:w], ALU.add, reads=["pb5", "s0t"], writes=["rnt"])
                self.recip(rnt[:, 0:w], rnt[:, 0:w], reads=["rnt"], writes=["rnt"])
                for fc in range(32):
                    s = fi % 2
                    fi += 1
                    P.dma(ft[s][:, 0, :, :], cd["dft_fwd"][fc * 128:(fc + 1) * 128, :].rearrange("p (a b) -> p a b", b=128), writes=[f"ft{s}"])
                    P.dma(ft[s][:, 1, :, :], cd["dft_fwd"][(32 + fc) * 128:(33 + fc) * 128, :].rearrange("p (a b) -> p a b", b=128), writes=[f"ft{s}"])
                    for d in range(2):
                        for ri in range(2):
                            pid = d * 2 + ri
                            for tc in range(32):
                                self.mm(self.pb[pid][:, 0:w], ft[s][:, ri, tc, :], hdb[d][:, tc, 0:w], tc == 0, tc == 31, reads=[f"ft{s}", f"hdb{d}_{tc}"], writes=[f"pb{pid}"])
                    g_re, g_im = gre[s], gim[s]
                    self.cp("act", s0t[:, 0:w], self.pb[0][:, 0:w], reads=["pb0"], writes=["s0t"])
                    self.tt("dve", g_re[:, 0:w], self.pb[2][:, 0:w], s0t[:, 0:w], ALU.add, reads=["pb2", "s0t"], writes=[f"gre{s}"])
                    self.tt("pool", g_re[:, 0:w], g_re[:, 0:w], rnt[:, 0:w], ALU.mult, reads=[f"gre{s}", "rnt"], writes=[f"gre{s}"])
                    self.cp("act", s1t[:, 0:w], self.pb[3][:, 0:w], reads=["pb3"], writes=["s1t"])
                    self.tt("dve", g_im[:, 0:w], self.pb[1][:, 0:w], s1t[:, 0:w], ALU.subtract, reads=["pb1", "s1t"], writes=[f"gim{s}"])
                    if fc == 0:
                        self.tt("dve", g_im[0:1, 0:w], self.pb[1][0:1, 0:w], s1t[0:1, 0:w], ALU.add, reads=["pb1", "s1t", f"gim{s}"], writes=[f"gim{s}"])
                    self.tt("pool", g_im[:, 0:w], g_im[:, 0:w], rnt[:, 0:w], ALU.mult, reads=[f"gim{s}", "rnt"], writes=[f"gim{s}"])
                    P.dma(GA[o][fc * 128:(fc + 1) * 128, c0:c1], g_re[:, 0:w], reads=[f"gre{s}"], writes=[f"GA{o}"], eng="act")
                    P.dma(GB[o][fc * 128:(fc + 1) * 128, c0:c1], g_im[:, 0:w], reads=[f"gim{s}"], writes=[f"GB{o}"], eng="act")
        P.barrier()
        A.off = base
        w_u = A.alloc([8, 2304], BF16)
        sv = A.off
        self._wstg = [A.alloc([1024], F32) for _ in range(2)]
        self._wstg_i = 0
        self.load_w_bf16(w_u, I["w_in_c"], 2304, "w_in")
        cst = A.alloc([2304], F32)
        convT = A.alloc([18, 4], F32)
        for k in range(3):
            P.dma(cst[k:k + 1, :], I["conv_w_c"][k:k + 1, :], writes=["cst"])
        P.dma(cst[3:4, :], I["conv_b_c"][0:1, :], writes=["cst"])
        for ch in range(18):
            ps = self.pb[ch % 2]
            self.tp(ps[:, 0:4], cst[0:4, ch * 128:(ch + 1) * 128], self.ident_f[0:4, 0:4], reads=["cst", "ident_f"], writes=[f"pb{ch % 2}"])
            self.cp("dve", convT[:, ch, :], ps[:, 0:4], reads=[f"pb{ch % 2}"], writes=["convT"])
        P.barrier()
        A.off = sv
        convT2 = A.alloc([18, 4], F32)
        self.cp("pool", convT2, convT, reads=[], writes=["convT2"])
        P.barrier()
        convT = convT2
        xh = [A.alloc([8, 514], BF16) for _ in range(2)]
        xhh = [A.alloc([8, 2], BF16) for _ in range(2)]
        ub = [A.alloc([514], F32) for _ in range(2)]
        acc = [A.alloc([TT], F32) for _ in range(2)]
        ucb = [A.alloc([TT], BF16) for _ in range(2)]
        tmrow = [A.alloc([4, 2304], BF16) for _ in range(2)]
        for b in range(NB):
            for t in range(8):
                tg = b * SEQ + t * TT
                xs = t % 2
                lo = 0 if t == 0 else -1
                hi = 512 if t == 7 else 513
                if t == 0:
                    P.op("pool", lambda e, xs=xs: e.memset(xh[xs][:, :, 0:1], 0.0), writes=[f"xh{xs}"])
                if t == 7:
                    P.op("pool", lambda e, xs=xs: e.memset(xh[xs][:, :, 513:514], 0.0), writes=[f"xh{xs}"])
                rk = self.xkeys("X0b", tg, TT)
                for c in range(8):
                    P.dma(xh[xs][:, c, 1 + lo:1 + hi], self.X0b[c * 128:(c + 1) * 128, tg + lo:tg + hi], reads=rk, writes=[f"xh{xs}"])
                self.cp("pool", xhh[xs][:, :, 0:1], xh[xs][:, :, 0:1], reads=[f"xh{xs}"], writes=[f"xhh{xs}"])
                self.cp("pool", xhh[xs][:, :, 1:2], xh[xs][:, :, 513:514], reads=[f"xh{xs}"], writes=[f"xhh{xs}"])
                trs = (b * 8 + t) % 2
                for ch in range(18):
                    s = ch % 2
                    pm, ph = self.pb[s], self.pb[2 + s]
                    for c in range(8):
                        self.mm(pm, w_u[:, c, ch * 128:(ch + 1) * 128], xh[xs][:, c, 1:513], c == 0, c == 7, reads=["w_in", f"xh{xs}"], writes=[f"pb{s}"])
                    for c in range(8):
                        self.mm(ph[:, 0:2], w_u[:, c, ch * 128:(ch + 1) * 128], xhh[xs][:, c, :], c == 0, c == 7, reads=["w_in", f"xhh{xs}"], writes=[f"pb{2 + s}"])
                    self.cp("act", ub[s][:, 1:513], pm, reads=[f"pb{s}"], writes=[f"ub{s}"])
                    self.cp("dve", ub[s][:, 0:1], ph[:, 0:1], reads=[f"pb{2 + s}"], writes=[f"ub{s}"])
                    self.cp("dve", ub[s][:, 513:514], ph[:, 1:2], reads=[f"pb{2 + s}"], writes=[f"ub{s}"])
                    self.ts("dve", acc[s], ub[s][:, 0:512], convT[:, ch, 0:1], convT[:, ch, 3:4], ALU.mult, ALU.add, reads=[f"ub{s}", "convT2"], writes=[f"acc{s}"])
                    self.stt(acc[s], ub[s][:, 1:513], convT[:, ch, 1:2], acc[s], ALU.mult, ALU.add, reads=[f"ub{s}", "convT2", f"acc{s}"], writes=[f"acc{s}"])
                    self.stt(ucb[s], ub[s][:, 2:514], convT[:, ch, 2:3], acc[s], ALU.mult, ALU.add, reads=[f"ub{s}", "convT2", f"acc{s}"], writes=[f"ucb{s}"])
                    pt = self.pb[4 + s].bitcast(BF16)
                    for blk in range(4):
                        self.tp(pt[:, blk * 128:(blk + 1) * 128], ucb[s][:, blk * 128:(blk + 1) * 128], ident_b, reads=[f"ucb{s}", "ident_b"], writes=[f"pb{4 + s}"])
                    self.cp("act" if ch % 2 else "dve", tmrow[trs][:, :, ch * 128:(ch + 1) * 128], pt[:, 0:512].rearrange("p (a b) -> p a b", a=4), reads=[f"pb{4 + s}"], writes=[f"tmrow{trs}"])
                for blk in range(4):
                    for part in range(3):
                        P.dma(TM[part][tg + blk * 128: tg + (blk + 1) * 128, :], tmrow[trs][:, blk, part * 768:(part + 1) * 768], reads=[f"tmrow{trs}"], writes=[f"TM{part}"], eng="act")
        P.barrier()
        A.off = base
        zbuf = [A.alloc([32, TT], BF16) for _ in range(2)]
        Ysp = A.alloc([64, TT], BF16)
        ft = [A.alloc([2, 32, 128], BF16) for _ in range(2)]
        sre = [A.alloc([TT], F32) for _ in range(2)]
        sim = [A.alloc([TT], F32) for _ in range(2)]
        tq = [A.alloc([TT], F32) for _ in range(4)]
        ga = [A.alloc([256], F32) for _ in range(2)]
        gbb = [A.alloc([256], F32) for _ in range(2)]
        xg = [A.alloc([TT], BF16) for _ in range(2)]
        lb = A.alloc([256], F32)
        zo = [A.alloc([TT], BF16) for _ in range(2)]
        mixTb = A.alloc([2, 2, TT], BF16)
        fi = 0
        for cg in range(3):
            for o in range(2):
                zin, zout = zbuf[o], zbuf[1 - o]
                zk = f"zb{o}_"
                zok = f"zb{1 - o}_"
                if o == 0:
                    for b in range(NB):
                        for tc in range(32):
                            P.dma(zin[:, tc, b * 256:(b + 1) * 256], TM[0][b * SEQ + tc * 128: b * SEQ + (tc + 1) * 128, cg * 256:(cg + 1) * 256], writes=[f"{zk}{tc}"])
                P.dma(lb, I["long_bias_c"][o:o + 1, cg * 256:(cg + 1) * 256].partition_broadcast(128), writes=["lb"])
                for fc in range(32):
                    s = fi % 2
                    fi += 1
                    P.dma(ft[s][:, 0, :, :], cd["dft_fwd"][fc * 128:(fc + 1) * 128, :].rearrange("p (a b) -> p a b", b=128), writes=[f"ft{s}"])
                    P.dma(ft[s][:, 1, :, :], cd["dft_fwd"][(32 + fc) * 128:(33 + fc) * 128, :].rearrange("p (a b) -> p a b", b=128), writes=[f"ft{s}"])
                    P.dma(ga[s], GA[o][fc * 128:(fc + 1) * 128, cg * 256:(cg + 1) * 256], writes=[f"ga{s}"])
                    P.dma(gbb[s], GB[o][fc * 128:(fc + 1) * 128, cg * 256:(cg + 1) * 256], writes=[f"gb{s}"])
                    pr, pi_ = self.pb[s * 2], self.pb[s * 2 + 1]
                    for tc in range(32):
                        self.mm(pr, ft[s][:, 0, tc, :], zin[:, tc, :], tc == 0, tc == 31, reads=[f"ft{s}", f"{zk}{tc}"], writes=[f"pb{s * 2}"])
                    for tc in range(32):
                        self.mm(pi_, ft[s][:, 1, tc, :], zin[:, tc, :], tc == 0, tc == 31, reads=[f"ft{s}", f"{zk}{tc}"], writes=[f"pb{s * 2 + 1}"])
                    self.cp("act", sre[s], pr, reads=[f"pb{s * 2}"], writes=[f"sre{s}"])
                    self.cp("act", sim[s], pi_, reads=[f"pb{s * 2 + 1}"], writes=[f"sim{s}"])
                    gA = ga[s].unsqueeze(1).to_broadcast([128, 2, 256])
                    gB = gbb[s].unsqueeze(1).to_broadcast([128, 2, 256])
                    v3 = lambda ap: ap.rearrange("p (b c) -> p b c", b=2)
                    t1, t2, t3, t4 = tq
                    self.tt("dve", v3(t1), v3(sre[s]), gA, ALU.mult, reads=[f"sre{s}", f"ga{s}"], writes=["tq0"])
                    self.tt("pool", v3(t2), v3(sim[s]), gB, ALU.mult, reads=[f"sim{s}", f"gb{s}"], writes=["tq1"])
                    self.tt("dve", v3(t3), v3(sre[s]), gB, ALU.mult, reads=[f"sre{s}", f"gb{s}"], writes=["tq2"])
                    self.tt("pool", v3(t4), v3(sim[s]), gA, ALU.mult, reads=[f"sim{s}", f"ga{s}"], writes=["tq3"])
                    self.tt("dve", Ysp[:, fc, :], t1, t2, ALU.subtract, reads=["tq0", "tq1"], writes=[f"Y{fc}"])
                    self.tt("dve", Ysp[:, 32 + fc, :], t3, t4, ALU.add, reads=["tq2", "tq3"], writes=[f"Y{32 + fc}"])
                    if fc == 0:
                        self.tt("dve", v3(Ysp[0:1, 0, :]), v3(sre[s][0:1, :]), gA[0:1], ALU.mult, reads=[f"sre{s}", f"ga{s}", "Y0"], writes=["Y0"])
                        self.tt("dve", v3(Ysp[0:1, 32, :]), v3(sim[s][0:1, :]), gB[0:1], ALU.mult, reads=[f"sim{s}", f"gb{s}", "Y32"], writes=["Y32"])
                for tch in range(32):
                    s = fi % 2
                    fi += 1
                    ftv = ft[s].rearrange("p a b c -> p (a b) c")
                    P.dma(ftv, cd["dft_inv"][tch * 128:(tch + 1) * 128, :].rearrange("p (a b) -> p a b", b=128), writes=[f"ft{s}"])
                    for b in range(NB):
                        P.dma(xg[s][:, b * 256:(b + 1) * 256], TM[1 + o][b * SEQ + tch * 128: b * SEQ + (tch + 1) * 128, cg * 256:(cg + 1) * 256], writes=[f"xg{s}"])
                    py = self.pb[4 + s]
                    for fch in range(64):
                        self.mm(py, ftv[:, fch, :], Ysp[:, fch, :], fch == 0, fch == 63, reads=[f"ft{s}", f"Y{fch}"], writes=[f"pb{4 + s}"])
                    v3 = lambda ap: ap.rearrange("p (b c) -> p b c", b=2)
                    lbb = lb.unsqueeze(1).to_broadcast([128, 2, 256])
                    ta, tb_ = tq[s * 2], tq[s * 2 + 1]
                    self.tt("pool", v3(ta), v3(zin[:, tch, :]), lbb, ALU.mult, reads=[f"{zk}{tch}", "lb"], writes=[f"tq{s * 2}"])
                    self.tt("dve", tb_, py, ta, ALU.add, reads=[f"pb{4 + s}", f"tq{s * 2}"], writes=[f"tq{s * 2 + 1}"])
                    if o == 0:
                        self.tt("pool", zout[:, tch, :], tb_, xg[s], ALU.mult, reads=[f"tq{s * 2 + 1}", f"xg{s}"], writes=[f"{zok}{tch}"])
                    else:
                        self.tt("pool", zo[s], tb_, xg[s], ALU.mult, reads=[f"tq{s * 2 + 1}", f"xg{s}"], writes=[f"zo{s}"])
                        pt = self.pb[6 + s].bitcast(BF16)
                        for b in range(NB):
                            for cb in range(2):
                                k4 = b * 2 + cb
                                self.tp(pt[:, k4 * 128:(k4 + 1) * 128], zo[s][:, b * 256 + cb * 128: b * 256 + (cb + 1) * 128], ident_b, reads=[f"zo{s}", "ident_b"], writes=[f"pb{6 + s}"])
                        q4 = tch % 4
                        for b in range(NB):
                            self.cp("act" if b else "dve", mixTb[:, :, b, q4 * 128:(q4 + 1) * 128], pt[:, b * 256:(b + 1) * 256].rearrange("p (a c) -> p a c", a=2), reads=[f"pb{6 + s}"], writes=["mixTb"])
                        if q4 == 3:
                            for b in range(NB):
                                for cb in range(2):
                                    t0 = b * SEQ + (tch // 4) * TT
                                    P.dma(mixT[cg * 256 + cb * 128: cg * 256 + (cb + 1) * 128, t0:t0 + TT], mixTb[:, cb, b, :], reads=["mixTb"], writes=[f"mixT{cg}_{cb}"], eng="act")
        P.barrier()
        A.off = base
        w_qm = A.alloc([8, 256], BF16)
        w_out = A.alloc([8, D], BF16)
        sv = A.off
        self._wstg = [A.alloc([1024], F32) for _ in range(2)]
        self._wstg_i = 0
        self.load_w_bf16(w_qm, I["w_in_c"], 256, "w_in", col0=2304)
        self.load_w_bf16(w_out, I["w_out"][li * D:(li + 1) * D, :], D, "w_out")
        P.barrier()
        A.off = sv
        self.w_in, self.w_out, self.qm_col0 = w_qm, w_out, 0
        T = self.attn_tail_bufs()
        catk = [f"cat{c}" for c in range(8)]
        for b in range(NB):
            for t in range(8):
                tg = b * SEQ + t * TT
                slot = t % 2
                self.load_x0(T, tg, slot)
                for ch in range(6):
                    P.dma(T["cat"][:, ch, :], mixT[ch * 128:(ch + 1) * 128, tg:tg + TT], writes=[catk[ch]])
                self.tail(T, li, b, tg, slot)

    def moe(self, li):
        P, A, I = self.P, self.A, self.I
        A.reset()
        last = (self.mode == "full" and li == CFG["layers"][-1])
        self.eps_t = A.alloc([1], F32)
        P.op("pool", lambda e: e.memset(self.eps_t, LN_EPS), writes=["ln_eps"])
        rw = A.alloc([8, NE], F32)
        rb = A.alloc([NE], F32)
        b1T = A.alloc([16, NE], F32)
        b2t = A.alloc([D], F32)
        for c in range(8):
            P.dma(rw[:, c, :], I["router_w"][li * D + c * 128: li * D + (c + 1) * 128, :], writes=["rw"])
        P.dma(rb, I["router_b"][li:li + 1, :].partition_broadcast(128), writes=["rb"])
        P.dma(b2t[0:NE, :], I["moe_b2"][li * NE:(li + 1) * NE, :], writes=["b2t"])
        b1s = A.alloc([2 * DE], F32)
        P.dma(b1s[0:NE, :], I["moe_b1"][li * NE:(li + 1) * NE, :], writes=["b1s"])
        for g in range(2):
            ps = self.pb[6 + g]
            for k in range(8):
                jch = g * 8 + k
                self.tp(ps[:, k * 32:(k + 1) * 32], b1s[0:NE, jch * 128:(jch + 1) * 128], self.ident_f[0:NE, 0:NE], reads=["b1s", "ident_f"], writes=[f"pb{6+g}"])
            self.cp("dve", b1T[:, g * 8:(g + 1) * 8, :], ps[:, 0:256].rearrange("p (k e) -> p k e", k=8), reads=[f"pb{6+g}"], writes=["b1T"])
        x1b = A.alloc([8, 1024], BF16)
        yacc = A.alloc([8, 1024], F32)
        w1h = [A.alloc([8, 1024], BF16) for _ in range(2)]
        w2h = [A.alloc([4, 1024], BF16) for _ in range(2)]
        aT = [A.alloc([4, TT], BF16) for _ in range(2)]
        gb = [A.alloc([TT], F32) for _ in range(3)]
        ub = [A.alloc([TT], F32) for _ in range(3)]
        cB = [A.alloc([1024], F32) for _ in range(2)]
        cT = A.alloc([1024], F32)
        cTs = A.alloc([1024], F32)
        rt = {k: A.alloc([NE], F32) for k in ("lg", "mask", "ex")}
        m8 = A.alloc([8], F32)
        sm = {k: A.alloc([1], F32) for k in ("nmax", "ssum", "rs")}
        ln = self.ln_tmp()
        xst = A.alloc([8, TT], F32)
        outb = A.alloc([8, TT], BF16)
        ne = CFG["n_experts"]
        X1f_v = self.X1f.rearrange("(c p) t -> p c t", p=128)
        X1b_v = self.X1b.rearrange("(c p) t -> p c t", p=128)
        units = [(e, hf) for e in range(ne) for hf in range(2)]

        def load_unit(tk, ui):
            e, hf = units[ui]
            s = ui % 2
            row0 = (e * 2 + hf) * 128
            P.dma(w1h[s], self.WB1[li][row0:row0 + 128, :].rearrange("p (c n) -> p c n", c=8), writes=[f"w1h{s}"])
            P.dma(w2h[s], self.WB2[li][row0:row0 + 128, :].rearrange("p (c n) -> p c n", c=4), writes=[f"w2h{s}"])

        def load_cb(tk, e):
            tg0 = tk * 1024
            P.dma(cB[e % 2], self.CTs[li * NE + e: li * NE + e + 1, tg0:tg0 + 1024].partition_broadcast(128), reads=[f"CTs{tk}"], writes=[f"cB{e % 2}"])

        for tk in range(NT // 1024):
            tg0 = tk * 1024
            self.fm_load(x1b, self.X1b, tg0, 1024, "x1b")
            for blk in range(8):
                if blk % 4 == 0:
                    self.fm_load(xst, self.X1f, tg0 + (blk // 4) * TT, TT, [f"xst{c}" for c in range(8)])
                ps = self.pb[6]
                for c in range(8):
                    self.mm(ps[:, 0:NE], xst[:, c, (blk % 4) * 128:(blk % 4 + 1) * 128], rw[:, c, :], c == 0, c == 7, reads=[f"xst{c}", "rw"], writes=["pb6"])
                self.tt("dve", rt["lg"], ps[:, 0:NE], rb, ALU.add, reads=["pb6", "rb"], writes=["r_lg"])
                P.op("dve", lambda e: e.max(out=m8, in_=rt["lg"]), reads=["r_lg"], writes=["r_m8"])
                self.ts("dve", sm["nmax"], m8[:, 0:1], -1.0, None, ALU.mult, None, reads=["r_m8"], writes=["r_nmax"])
                self.ts("dve", rt["mask"], rt["lg"], m8[:, 3:4], None, ALU.is_ge, None, reads=["r_lg", "r_m8"], writes=["r_mask"])
                self.act(rt["ex"], rt["lg"], AF.Exp, reads=["r_lg", "r_nmax"], writes=["r_ex"], bias=sm["nmax"][:, 0:1])
                self.tt("dve", rt["ex"], rt["ex"], rt["mask"], ALU.mult, reads=["r_ex", "r_mask"], writes=["r_ex"])
                P.op("dve", lambda e: e.reduce_sum(out=sm["ssum"], in_=rt["ex"], axis=AX.X), reads=["r_ex"], writes=["r_ssum"])
                self.recip(sm["rs"], sm["ssum"], reads=["r_ssum"], writes=["r_rs"])
                self.ts("dve", rt["ex"], rt["ex"], sm["rs"][:, 0:1], None, ALU.mult, None, reads=["r_ex", "r_rs"], writes=["r_ex"])
                pt = self.pb[7]
                self.tp(pt[0:NE, 0:128], rt["ex"], self.ident_f, reads=["r_ex", "ident_f"], writes=["pb7"])
                self.cp("dve", cT[0:NE, blk * 128:(blk + 1) * 128], pt[0:NE, 0:128], reads=["pb7"], writes=["cT"])
                self.act(cTs[0:NE, blk * 128:(blk + 1) * 128], pt[0:NE, 0:128], AF.Copy, reads=["pb7"], writes=["cTs"], scale=1.0 / 1.702)
            P.dma(self.CTs[li * NE:(li + 1) * NE, tg0:tg0 + 1024], cTs[0:NE, :], reads=["cTs"], writes=[f"CTs{tk}"], eng="act")
            for fo in range(8):
                for st in range(2):
                    pid = 4 + (fo * 2 + st) % 2
                    ps = self.pb[pid]
                    self.mm(ps, b2t[0:NE, fo * 128:(fo + 1) * 128], cT[0:NE, st * TT:(st + 1) * TT], True, True, reads=["b2t", "cT"], writes=[f"pb{pid}"])
                    self.cp("act", yacc[:, fo, st * TT:(st + 1) * TT], ps, reads=[f"pb{pid}"], writes=[f"y{fo}_{st}"])
            load_unit(tk, 0)
            load_cb(tk, 0)
            pend = []
            pair_i = 0
            for ui, (e, hf) in enumerate(units):
                if ui + 1 < len(units):
                    load_unit(tk, ui + 1)
                    if units[ui + 1][1] == 0:
                        load_cb(tk, units[ui + 1][0])
                ws = ui % 2
                cb = cB[e % 2]
                for st in range(2):
                    a_t = aT[st]
                    for jj in range(4):
                        pi = pair_i % 2
                        bi = pair_i % 3
                        pair_i += 1
                        pg, pu = self.pb[pi * 2], self.pb[pi * 2 + 1]
                        pgk, puk = f"pb{pi * 2}", f"pb{pi * 2 + 1}"
                        for c in range(8):
                            self.mm(pg, w1h[ws][:, c, jj * 128:(jj + 1) * 128], x1b[:, c, st * TT:(st + 1) * TT], c == 0, c == 7, reads=[f"w1h{ws}", "x1b"], writes=[pgk])
                        for c in range(8):
                            self.mm(pu, w1h[ws][:, c, 512 + jj * 128: 512 + (jj + 1) * 128], x1b[:, c, st * TT:(st + 1) * TT], c == 0, c == 7, reads=[f"w1h{ws}", "x1b"], writes=[puk])
                        jg = hf * 4 + jj
                        g_, u_ = gb[bi], ub[bi]
                        gk, uk = f"gb{bi}", f"ub{bi}"
                        self.ts("dve", g_, pg, b1T[:, jg, e:e + 1], 7.0, ALU.add, ALU.min, reads=[pgk, "b1T"], writes=[gk])
                        self.act(u_, pu, AF.Identity, reads=[puk, "b1T"], writes=[uk], bias=b1T[:, 8 + jg, e:e + 1])
                        self.act(g_, g_, AF.Silu, reads=[gk], writes=[gk], scale=1.702)
                        self.ts("dve", u_, u_, -7.0, 7.0, ALU.max, ALU.min, reads=[uk], writes=[uk])
                        self.tt("pool", g_, g_, cb[:, st * TT:(st + 1) * TT], ALU.mult, reads=[gk, f"cB{e % 2}"], writes=[gk])
                        for f_ in pend:
                            f_()
                        pend = [lambda a_t=a_t, jj=jj, u_=u_, g_=g_, uk=uk, gk=gk, st=st: self.stt(a_t[:, jj, :], u_, 1.0, g_, ALU.add, ALU.mult, reads=[uk, gk], writes=[f"aT{st}_{jj}"])]
                    for f_ in pend:
                        f_()
                    pend = []
                    for fo in range(8):
                        pid = 4 + fo % 2
                        py = self.pb[pid]
                        for jj in range(4):
                            self.mm(py, w2h[ws][:, jj, fo * 128:(fo + 1) * 128], a_t[:, jj, :], jj == 0, jj == 3, reads=[f"w2h{ws}", f"aT{st}_{jj}"], writes=[f"pb{pid}"])
                        ys = yacc[:, fo, st * TT:(st + 1) * TT]
                        self.tt("dve", ys, py, ys, ALU.add, reads=[f"pb{pid}", f"y{fo}_{st}"], writes=[f"y{fo}_{st}"])
            for st in range(2):
                tg = tg0 + st * TT
                self.fm_load(xst, self.X1f, tg, TT, [f"xst{c}" for c in range(8)])
                z = yacc[:, :, st * TT:(st + 1) * TT]
                for fo in range(8):
                    self.stt(z[:, fo, :], xst[:, fo, :], ALPHA, z[:, fo, :], ALU.mult, ALU.add, reads=[f"xst{fo}", f"y{fo}_{st}"], writes=[f"y{fo}_{st}"])
                self.layernorm(z, "ln2", li, outb, [f"y{c}_{st}" for c in range(8)], [f"outb{c}" for c in range(8)], ln)
                if not last:
                    self.fm_store(self.X0f, tg, TT, z, [f"y{c}_{st}" for c in range(8)])
                    self.fm_store(self.X0b, tg, TT, outb, [f"outb{c}" for c in range(8)])
                else:
                    for k in range(4):
                        for g in range(2):
                            pid = 6 + g
                            ps = self.pb[pid]
                            for cc in range(4):
                                c = g * 4 + cc
                                self.tp(ps[:, cc * 128:(cc + 1) * 128], z[:, c, k * 128:(k + 1) * 128], self.ident_f, reads=[f"y{c}_{st}", "ident_f"], writes=[f"pb{pid}"])
                            self.cp("dve" if g else "act", xst[:, (k % 2) * 2 + g, :], ps, reads=[f"pb{pid}"], writes=[f"xst{(k % 2) * 2 + g}"])
                        src = xst[:, (k % 2) * 2:(k % 2) * 2 + 2, :].rearrange("p a b -> p (a b)")
                        P.dma(self.out[tg + k * 128: tg + (k + 1) * 128, :], src, reads=[f"xst{(k % 2) * 2}", f"xst{(k % 2) * 2 + 1}"], writes=[self.key("out")], eng="act")
            if self.dbg is not None and CFG["dump"] == ("x2", li):
                pass


_CONSTS = None


def prepare_inputs(inputs):
    global _CONSTS
    if _CONSTS is None:
        _CONSTS = host_constants()
    C = _CONSTS
    f = lambda a: np.ascontiguousarray(np.asarray(a, dtype=np.float32))
    shared = {}
    shared["w_in_a"] = f(inputs["w_in_a"]).reshape(2 * D, 2560)
    rp = f(inputs["rpb_a"])[:, :, ::-1, ::-1]
    rpp = np.zeros((2, 12, 24, 31), np.float32)
    rpp[:, :, 5:20, :] = rp
    shared["rpb"] = rpp.reshape(24, 24 * 31)
    shared["w_in_b"] = f(inputs["w_in_b"]).reshape(D, 1536)
    shared["q_norm_b"] = f(inputs["q_norm_b"]).reshape(64, 1)
    shared["k_norm_b"] = f(inputs["k_norm_b"]).reshape(64, 1)
    shared["w_in_c"] = f(inputs["w_in_c"]).reshape(D, 2560)
    shared["conv_w_c"] = f(inputs["conv_w_c"]).reshape(3, 2304)
    shared["conv_b_c"] = f(inputs["conv_b_c"]).reshape(1, 2304)
    shared["filt_w1"] = f(inputs["filt_w1"]).reshape(33, 64)
    shared["filt_b1"] = f(inputs["filt_b1"]).reshape(64, 1)
    shared["filt_w2"] = f(inputs["filt_w2"]).reshape(64, 64)
    shared["filt_b2"] = f(inputs["filt_b2"]).reshape(64, 1)
    shared["filt_w3"] = f(inputs["filt_w3"]).reshape(64, 64)
    shared["filt_b3"] = f(inputs["filt_b3"]).reshape(64, 1)
    shared["filt_w4"] = f(inputs["filt_w4"]).reshape(64, 3072)
    shared["filt_b4"] = f(inputs["filt_b4"]).reshape(1, 3072)
    shared["filt_freq"] = f(inputs["filt_freq"]).reshape(64, 1)
    shared["long_bias_c"] = f(inputs["long_bias_c"]).reshape(2, 768)
    shared["w_mem_kv"] = f(inputs["w_mem_kv"]).reshape(DEPTH * D, 512)
    shared["w_out"] = f(inputs["w_out"]).reshape(DEPTH * D, D)
    for nm in ("ln1_g", "ln1_b", "ln2_g", "ln2_b"):
        shared[nm] = f(inputs[nm]).reshape(DEPTH * 8, 128)
    shared["router_w"] = f(inputs["router_w"]).reshape(DEPTH * D, NE)
    shared["router_b"] = f(inputs["router_b"]).reshape(DEPTH, NE)
    shared["moe_w1"] = f(inputs["moe_w1"]).reshape(DEPTH * NE * D, 2 * DE)
    shared["moe_w2"] = f(inputs["moe_w2"]).reshape(DEPTH * NE * DE, D)
    shared["moe_b1"] = f(inputs["moe_b1"]).reshape(DEPTH * NE, 2 * DE)
    shared["moe_b2"] = f(inputs["moe_b2"]).reshape(DEPTH * NE, D)
    for k, v in C.items():
        if not k.startswith("_"):
            shared["c_" + k] = v
    x = f(inputs["x"])
    mem = f(inputs["mem"])
    maps = []
    for c in range(NCORES):
        m = dict(shared)
        m["x"] = x[c * NB:(c + 1) * NB].reshape(NT, D)
        m["mem"] = mem[c * NB:(c + 1) * NB].reshape(NB * 256, D)
        maps.append(m)
    return maps


_PROGS = {}


def _prog(mode):
    if mode not in _PROGS:
        bld = Builder(_CONSTS, mode)
        nc = bld.build()
        _PROGS[mode] = (nc, list(bld.used_inputs))
    return _PROGS[mode]


def _launch(mode, shared, percore, ncores):
    nc, used = _prog(mode)
    maps = []
    for c in range(ncores):
        m = {}
        for k in used:
            m[k] = percore[c][k] if k in percore[c] else shared[k]
        maps.append(m)
    res = run_bass_kernel_spmd(nc, maps, core_ids=list(range(ncores)))
    return res.results


def _roll(a, n):
    return a if n == 0 else np.ascontiguousarray(np.roll(a, -n, axis=0))


def kernel(**inputs):
    maps = prepare_inputs(inputs)
    ncores = CFG.get("ncores", NCORES)
    shared = {k: v for k, v in maps[0].items() if k not in ("x", "mem")}
    percore = [{"x": maps[c]["x"], "mem": maps[c]["mem"]} for c in range(ncores)]
    if CFG.get("fused"):
        bld = Builder(_CONSTS)
        nc = bld.build()
        res = run_bass_kernel_spmd(nc, [{k: m[k] for k in bld.used_inputs} for m in maps[:ncores]], core_ids=list(range(ncores)))
        outs = [r["out"].reshape(NB, SEQ, D) for r in res.results]
        return np.concatenate(outs, axis=0).astype(np.float32)
    r = _launch("pro", shared, percore, ncores)
    for c in range(ncores):
        percore[c]["X0f"], percore[c]["X0b"] = r[c]["X0f"], r[c]["X0b"]
    lnn = ("ln1_g", "ln1_b", "ln2_g", "ln2_b")
    for li in range(DEPTH):
        kind = li % 3
        sh = dict(shared)
        if kind == 0:
            jj = li // 3
            sh["w_in_a"] = _roll(shared["w_in_a"], jj * D)
            sh["rpb"] = _roll(shared["rpb"], jj * 12)
            sh["w_mem_kv"] = _roll(shared["w_mem_kv"], li * D)
            sh["w_out"] = _roll(shared["w_out"], li * D)
            for nm in lnn:
                sh[nm] = _roll(shared[nm], li * 8)
        r = _launch(("mixA", "mixB", "mixC")[kind], sh, percore, ncores)
        for c in range(ncores):
            percore[c]["X1f"], percore[c]["X1b"] = r[c]["X1f"], r[c]["X1b"]
        sh = dict(shared)
        for nm in lnn:
            sh[nm] = _roll(shared[nm], li * 8)
        sh["router_w"] = _roll(shared["router_w"], li * D)
        sh["router_b"] = _roll(shared["router_b"], li)
        sh["moe_b1"] = _roll(shared["moe_b1"], li * NE)
        sh["moe_b2"] = _roll(shared["moe_b2"], li * NE)
        sh["moe_w1"] = shared["moe_w1"][li * NE * D:(li + 1) * NE * D]
        sh["moe_w2"] = shared["moe_w2"][li * NE * DE:(li + 1) * NE * DE]
        r = _launch("moe", sh, percore, ncores)
        for c in range(ncores):
            percore[c]["X0f"], percore[c]["X0b"] = r[c]["X0f"], r[c]["X0b"]
    outs = [np.ascontiguousarray(percore[c]["X0f"].T).reshape(NB, SEQ, D) for c in range(ncores)]
    return np.concatenate(outs, axis=0).astype(np.float32)
```
